# Optimizing a Trainium2 kernel written in Bass

```python
import math
import jax, jax.numpy as jnp
from jax import lax
import numpy as np

D_MODEL = 2048
BATCH = 2
SEQ = 4096
DEPTH = 2

GRID_W = 64
N_MIXERS = 2
N_SSD_LAYERS = (DEPTH + 1) // 2
N_NA_LAYERS = DEPTH // 2
EPS = 1e-6

SSD_EXPAND = 2
D_INNER = SSD_EXPAND * D_MODEL
SSD_HEAD_DIM = 64
SSD_HEADS = D_INNER // SSD_HEAD_DIM
SSD_GROUPS = 8
SSD_HEADS_PER_GROUP = SSD_HEADS // SSD_GROUPS
SSD_STATE = 128
SSD_CONV = 5
SSD_CHUNK = 128
CONV_DIM = D_INNER + 2 * SSD_GROUPS * SSD_STATE
SSD_IN_DIM = D_INNER + CONV_DIM + 2 * SSD_HEADS

NA_HEAD_DIM = 64
NA_HEADS = D_MODEL // NA_HEAD_DIM
WIN_H = 8
WIN_W = 16

MOE_GROUPS = 4
MOE_EXPERTS_PER_GROUP = 8
N_EXPERTS = MOE_GROUPS * MOE_EXPERTS_PER_GROUP
MOE_TOP_K = 2
MOE_D_FF = D_MODEL // 4
MOE_BLOCK = 128

kernel_name = 'bidir_hybrid_ssd_natten_hmoe'


def rmsnorm(x, w):
    xf = x.astype(jnp.float32)
    y = xf * lax.rsqrt(jnp.mean(xf * xf, axis=-1, keepdims=True) + EPS)
    return (y * w).astype(x.dtype)


def segsum(a):
    q = a.shape[-1]
    cs = jnp.cumsum(a, axis=-1)
    diff = cs[..., :, None] - cs[..., None, :]
    mask = jnp.tril(jnp.ones((q, q), dtype=bool))
    return jnp.where(mask, diff, -jnp.inf)


def ssd_chunked(xdt, a_dt, bm, cm):
    b, L, G, R, P = xdt.shape
    N = bm.shape[-1]
    Q = SSD_CHUNK
    nc = L // Q
    x_c = xdt.reshape(b, nc, Q, G, R, P)
    a_c = jnp.moveaxis(a_dt.reshape(b, nc, Q, G, R), 2, -1)
    b_c = bm.reshape(b, nc, Q, G, N)
    c_c = cm.reshape(b, nc, Q, G, N)
    a_cum = jnp.cumsum(a_c, axis=-1)
    l_mat = jnp.exp(segsum(a_c))
    cb = jnp.einsum('bclgn,bcsgn->bcgls', c_c, b_c)
    y_diag = jnp.einsum('bcgrls,bcsgrp->bclgrp', cb[:, :, :, None] * l_mat, x_c)
    decay_states = jnp.exp(a_cum[..., -1:] - a_cum)
    states = jnp.einsum('bclgn,bclgrp->bcgrpn', b_c,
                        x_c * jnp.moveaxis(decay_states, -1, 2)[..., None])
    a_last = jnp.moveaxis(a_cum[..., -1], 1, -1)
    decay_chunk = jnp.exp(segsum(jnp.pad(a_last, ((0, 0), (0, 0), (0, 0), (1, 0)))))
    states = jnp.concatenate([jnp.zeros_like(states[:, :1]), states], axis=1)
    states = jnp.einsum('bgrzc,bcgrpn->bzgrpn', decay_chunk, states)[:, :-1]
    y_off = jnp.einsum('bclgn,bcgrpn->bclgrp', c_c, states) * jnp.moveaxis(jnp.exp(a_cum), -1, 2)[..., None]
    return (y_diag + y_off).reshape(b, L, G, R, P)


def dwconv_centred(u, w, bias):
    k = w.shape[0]
    out = lax.conv_general_dilated(u, w[:, None, :], window_strides=(1,), padding=[(k // 2, k // 2)],
                                   dimension_numbers=('NWC', 'WIO', 'NWC'), feature_group_count=u.shape[-1])
    return out + bias


def ssd_mixer(h, w_in, conv_w, conv_b, a_log, dt_bias, d_skip, norm_w, w_out):
    b, L, _ = h.shape
    G, R, P, N = SSD_GROUPS, SSD_HEADS_PER_GROUP, SSD_HEAD_DIM, SSD_STATE
    proj = h @ w_in
    z = proj[..., :D_INNER]
    xbc = proj[..., D_INNER:D_INNER + CONV_DIM]
    dt_raw = proj[..., D_INNER + CONV_DIM:]
    xbc = jax.nn.silu(dwconv_centred(xbc, conv_w, conv_b))
    xs = xbc[..., :D_INNER].reshape(b, L, G, R, P)
    bm = xbc[..., D_INNER:D_INNER + G * N].reshape(b, L, G, N)
    cm = xbc[..., D_INNER + G * N:].reshape(b, L, G, N)
    dt = jax.nn.softplus(dt_raw.astype(jnp.float32).reshape(b, L, 2, SSD_HEADS) + dt_bias)
    a = -jnp.exp(a_log.astype(jnp.float32))
    y = xs * d_skip.reshape(G, R)[:, :, None]
    for d in range(2):
        dt_d = dt[:, :, d].reshape(b, L, G, R)
        a_dt = dt_d * a[d].reshape(G, R)
        xdt = xs * dt_d[..., None]
        bd, cd = bm, cm
        if d == 1:
            xdt, a_dt, bd, cd = (jnp.flip(xdt, 1), jnp.flip(a_dt, 1), jnp.flip(bd, 1), jnp.flip(cd, 1))
        y_d = ssd_chunked(xdt, a_dt, bd, cd)
        if d == 1:
            y_d = jnp.flip(y_d, 1)
        y = y + y_d.astype(y.dtype)
    yf = (y.reshape(b, L, D_INNER) * jax.nn.silu(z)).astype(jnp.float32).reshape(b, L, SSD_GROUPS, -1)
    yf = yf * lax.rsqrt(jnp.mean(yf * yf, axis=-1, keepdims=True) + EPS)
    yn = (yf.reshape(b, L, D_INNER) * norm_w).astype(h.dtype)
    return yn @ w_out


def na_mixer(h, w_qkv, rpb, w_o):
    b, L, _ = h.shape
    rows = L // GRID_W
    kh = min(WIN_H, rows)
    scale = NA_HEAD_DIM ** -0.5
    qkv = (h @ w_qkv).reshape(b, rows, GRID_W, 3, NA_HEADS, NA_HEAD_DIM)
    q = (qkv[:, :, :, 0] * scale).transpose(1, 0, 3, 2, 4)
    k = qkv[:, :, :, 1].transpose(0, 3, 1, 2, 4)
    v = qkv[:, :, :, 2].transpose(0, 3, 1, 2, 4)
    j = jnp.arange(GRID_W)
    c0 = jnp.clip(j - WIN_W // 2, 0, GRID_W - WIN_W)
    col_idx = c0[:, None] + jnp.arange(WIN_W)[None, :]
    dxi = col_idx - j[:, None] + (WIN_W - 1)

    def row_attend(args):
        r, q_r = args
        r0 = jnp.clip(r - WIN_H // 2, 0, rows - kh)
        k_rows = lax.dynamic_slice_in_dim(k, r0, kh, axis=2)
        v_rows = lax.dynamic_slice_in_dim(v, r0, kh, axis=2)
        k_win = k_rows[:, :, :, col_idx]
        v_win = v_rows[:, :, :, col_idx]
        s = jnp.einsum('bhjd,bhajkd->bhjak', q_r, k_win).astype(jnp.float32)
        dyi = r0 + jnp.arange(kh) - r + (WIN_H - 1)
        bias = rpb[:, dyi[None, :, None], dxi[:, None, :]]
        s = s + bias[None].astype(jnp.float32)
        p = jax.nn.softmax(s.reshape(b, NA_HEADS, GRID_W, kh * WIN_W), axis=-1)
        p = p.reshape(b, NA_HEADS, GRID_W, kh, WIN_W).astype(v.dtype)
        return jnp.einsum('bhjak,bhajkd->bhjd', p, v_win)

    o = lax.map(row_attend, (jnp.arange(rows), q))
    o = o.transpose(1, 0, 3, 2, 4).reshape(b, L, D_MODEL)
    return o @ w_o


def hier_moe(h, w_group, w_expert, w1, w3, w2):
    b, L, D = h.shape
    T = b * L
    xt = h.reshape(T, D)
    g_logits = (xt @ w_group).astype(jnp.float32)
    g_prob = jax.nn.softmax(g_logits, axis=-1)
    g_sel = jnp.argmax(g_logits, axis=-1)
    g_w = jnp.take_along_axis(g_prob, g_sel[:, None], axis=1)[:, 0]
    e_logits = (xt @ w_expert).astype(jnp.float32).reshape(T, MOE_GROUPS, MOE_EXPERTS_PER_GROUP)
    e_logits = jnp.take_along_axis(e_logits, g_sel[:, None, None], axis=1)[:, 0]
    e_prob = jax.nn.softmax(e_logits, axis=-1)
    top_p, top_i = lax.top_k(e_prob, MOE_TOP_K)
    top_p = top_p / jnp.sum(top_p, axis=-1, keepdims=True)
    weights = (g_w[:, None] * top_p).astype(h.dtype)
    expert_id = (g_sel[:, None] * MOE_EXPERTS_PER_GROUP + top_i).astype(jnp.int32)
    A = T * MOE_TOP_K
    eid = expert_id.reshape(A)
    tid = jnp.repeat(jnp.arange(T, dtype=jnp.int32), MOE_TOP_K)
    wts = weights.reshape(A)
    order = jnp.argsort(eid)
    eid_s, tid_s, w_s = eid[order], tid[order], wts[order]
    counts = jnp.zeros((N_EXPERTS,), jnp.int32).at[eid].add(1)
    padded = ((counts + MOE_BLOCK - 1) // MOE_BLOCK) * MOE_BLOCK
    start = jnp.cumsum(counts) - counts
    pend = jnp.cumsum(padded)
    pstart = pend - padded
    dest = pstart[eid_s] + (jnp.arange(A, dtype=jnp.int32) - start[eid_s])
    nb = (A + N_EXPERTS * (MOE_BLOCK - 1) + MOE_BLOCK - 1) // MOE_BLOCK
    P = nb * MOE_BLOCK
    tok_buf = jnp.full((P,), T, jnp.int32).at[dest].set(tid_s)
    w_buf = jnp.zeros((P,), h.dtype).at[dest].set(w_s)
    blk_expert = jnp.minimum(jnp.searchsorted(pend, jnp.arange(nb, dtype=jnp.int32) * MOE_BLOCK, side='right'),
                             N_EXPERTS - 1)
    x_pad = jnp.concatenate([xt, jnp.zeros((1, D), xt.dtype)], axis=0)
    xb = x_pad[tok_buf].reshape(nb, MOE_BLOCK, D)

    def run_block(args):
        xb_i, e = args
        return (jax.nn.silu(xb_i @ w1[e]) * (xb_i @ w3[e])) @ w2[e]

    yb = lax.map(run_block, (xb, blk_expert)).reshape(P, D)
    out = jax.ops.segment_sum(yb * w_buf[:, None], tok_buf, num_segments=T + 1)[:T]
    return out.reshape(b, L, D)


def setup_inputs(seed: int = 0) -> dict:
    key = jax.random.key(seed)
    ks = jax.random.split(key, 24)
    f32 = jnp.float32

    def nrm(k, shape, scale):
        return jax.random.normal(k, shape, f32) * scale

    x = nrm(ks[0], (BATCH, SEQ, D_MODEL), 1.0)
    c = nrm(ks[1], (BATCH, D_MODEL), 1.0)
    ada_w = nrm(ks[2], (DEPTH, D_MODEL, 6 * D_MODEL), 0.5 * D_MODEL ** -0.5)
    ada_b = nrm(ks[3], (DEPTH, 6 * D_MODEL), 0.02)
    norm_mix = 1.0 + nrm(ks[4], (DEPTH, D_MODEL), 0.02)
    norm_ffn = 1.0 + nrm(ks[5], (DEPTH, D_MODEL), 0.02)
    ssd_w_in = nrm(ks[6], (N_SSD_LAYERS, D_MODEL, SSD_IN_DIM), D_MODEL ** -0.5)
    ssd_conv_w = nrm(ks[7], (N_SSD_LAYERS, SSD_CONV, CONV_DIM), SSD_CONV ** -0.5)
    ssd_conv_b = nrm(ks[8], (N_SSD_LAYERS, CONV_DIM), 0.02)
    ssd_a_log = jnp.log(jax.random.uniform(ks[9], (N_SSD_LAYERS, 2, SSD_HEADS), f32, 1.0, 16.0))
    dt0 = jnp.exp(jax.random.uniform(ks[10], (N_SSD_LAYERS, 2, SSD_HEADS), f32, math.log(1e-3), math.log(1e-1)))
    ssd_dt_bias = dt0 + jnp.log(-jnp.expm1(-dt0))
    ssd_d = 1.0 + nrm(ks[11], (N_SSD_LAYERS, SSD_HEADS), 0.1)
    ssd_norm_w = 1.0 + nrm(ks[12], (N_SSD_LAYERS, D_INNER), 0.02)
    ssd_w_out = nrm(ks[13], (N_SSD_LAYERS, D_INNER, D_MODEL), D_INNER ** -0.5)
    na_w_qkv = nrm(ks[14], (N_NA_LAYERS, D_MODEL, 3 * D_MODEL), D_MODEL ** -0.5)
    na_rpb = nrm(ks[15], (N_NA_LAYERS, NA_HEADS, 2 * WIN_H - 1, 2 * WIN_W - 1), 0.1)
    na_w_o = nrm(ks[16], (N_NA_LAYERS, D_MODEL, D_MODEL), D_MODEL ** -0.5)
    moe_w_group = nrm(ks[17], (DEPTH, D_MODEL, MOE_GROUPS), D_MODEL ** -0.5)
    moe_w_expert = nrm(ks[18], (DEPTH, D_MODEL, N_EXPERTS), D_MODEL ** -0.5)
    moe_w1 = nrm(ks[19], (DEPTH, N_EXPERTS, D_MODEL, MOE_D_FF), D_MODEL ** -0.5)
    moe_w3 = nrm(ks[20], (DEPTH, N_EXPERTS, D_MODEL, MOE_D_FF), D_MODEL ** -0.5)
    moe_w2 = nrm(ks[21], (DEPTH, N_EXPERTS, MOE_D_FF, D_MODEL), MOE_D_FF ** -0.5)
    final_norm = 1.0 + nrm(ks[22], (D_MODEL,), 0.02)
    return {'x': x, 'c': c, 'ada_w': ada_w, 'ada_b': ada_b, 'norm_mix': norm_mix, 'norm_ffn': norm_ffn,
            'ssd_w_in': ssd_w_in, 'ssd_conv_w': ssd_conv_w, 'ssd_conv_b': ssd_conv_b, 'ssd_a_log': ssd_a_log,
            'ssd_dt_bias': ssd_dt_bias, 'ssd_d': ssd_d, 'ssd_norm_w': ssd_norm_w, 'ssd_w_out': ssd_w_out,
            'na_w_qkv': na_w_qkv, 'na_rpb': na_rpb, 'na_w_o': na_w_o,
            'moe_w_group': moe_w_group, 'moe_w_expert': moe_w_expert, 'moe_w1': moe_w1, 'moe_w3': moe_w3,
            'moe_w2': moe_w2, 'final_norm': final_norm}


def reference(x, c, ada_w, ada_b, norm_mix, norm_ffn, ssd_w_in, ssd_conv_w, ssd_conv_b, ssd_a_log,
              ssd_dt_bias, ssd_d, ssd_norm_w, ssd_w_out, na_w_qkv, na_rpb, na_w_o,
              moe_w_group, moe_w_expert, moe_w1, moe_w3, moe_w2, final_norm):
    c_act = jax.nn.silu(c)
    for i in range(DEPTH):
        mod = (c_act @ ada_w[i] + ada_b[i])[:, None, :]
        sh1, sc1, g1, sh2, sc2, g2 = jnp.split(mod, 6, axis=-1)
        h = rmsnorm(x, norm_mix[i]) * (1.0 + sc1) + sh1
        j = i // N_MIXERS
        if i % N_MIXERS == 0:
            y = ssd_mixer(h, ssd_w_in[j], ssd_conv_w[j], ssd_conv_b[j], ssd_a_log[j], ssd_dt_bias[j],
                          ssd_d[j], ssd_norm_w[j], ssd_w_out[j])
        else:
            y = na_mixer(h, na_w_qkv[j], na_rpb[j], na_w_o[j])
        x = x + g1 * y
        h = rmsnorm(x, norm_ffn[i]) * (1.0 + sc2) + sh2
        x = x + g2 * hier_moe(h, moe_w_group[i], moe_w_expert[i], moe_w1[i], moe_w3[i], moe_w2[i])
    return rmsnorm(x, final_norm)
```

```python
import contextlib
import numpy as np
import concourse.bass as bass
import concourse.mybir as mybir
from concourse.bass_utils import run_bass_kernel_spmd

F32 = mybir.dt.float32
BF16 = mybir.dt.bfloat16
I32 = mybir.dt.int32
ALU = mybir.AluOpType
AF = mybir.ActivationFunctionType
AX = mybir.AxisListType


class Sched:
    ENGS = ['pe', 'act', 'dve', 'pool', 'sp']

    def __init__(self, nc, ctx):
        self.nc = nc
        self.ctx = ctx
        self.q = {e: [] for e in self.ENGS}
        self.cnt = {e: 0 for e in self.ENGS}
        self.sems = {e: ctx.enter_context(nc.semaphore('s_' + e)) for e in self.ENGS}
        self.dsem = {}
        self.lastw = {}
        self.readers = {}
        self.seen = {e: {} for e in self.ENGS}
        self.nwaits = 0

    def _semh(self, key):
        if key[0] == 'e':
            return self.sems[key[1]]
        return self.dsem[key[1]][0]

    def op(self, eng, fn, reads=(), writes=(), dma=None, pe_acc=False, inc=16):
        deps = {}
        def add(sig):
            k, v, e = sig[:3]
            if pe_acc and eng == 'pe' and e == 'pe' and k[0] == 'e':
                return
            if deps.get(k, 0) < v:
                deps[k] = v
        for r in reads:
            if r in self.lastw:
                add(self.lastw[r])
        for w in writes:
            if w in self.lastw:
                add(self.lastw[w])
            for rd in self.readers.get(w, ()):
                add(rd)
        waits = []
        for k, v in deps.items():
            if self.seen[eng].get(k, 0) >= v:
                continue
            self.seen[eng][k] = v
            waits.append((k, v))
        if dma is not None:
            if dma not in self.dsem:
                self.dsem[dma] = [self.ctx.enter_context(self.nc.semaphore('d_' + dma)), 0]
            self.dsem[dma][1] += inc
            sig = (('d', dma), self.dsem[dma][1], eng, inc)
        else:
            self.cnt[eng] += 1
            sig = (('e', eng), self.cnt[eng], eng)
        for w in writes:
            self.lastw[w] = sig
            self.readers[w] = []
        for r in reads:
            if r not in writes:
                self.readers.setdefault(r, []).append(sig)
        self.q[eng].append((fn, waits, sig))
        self.nwaits += len(waits)

    def barrier(self):
        sigs = []
        for e in self.ENGS:
            if self.cnt[e] > 0:
                sigs.append((('e', e), self.cnt[e]))
        for name, (h, c) in self.dsem.items():
            if c > 0:
                sigs.append((('d', name), c))
        for e in self.ENGS:
            waits = []
            for k, v in sigs:
                if self.seen[e].get(k, 0) >= v:
                    continue
                self.seen[e][k] = v
                waits.append((k, v))
            if waits:
                self.q[e].append((None, waits, None))
        self.lastw = {}
        self.readers = {}

    def emit(self):
        nc = self.nc
        with nc.Block() as block:
            def mk(ename):
                def body(eng):
                    for fn, waits, sig in self.q[ename]:
                        for k, v in waits:
                            eng.wait_ge(self._semh(k), v)
                        if fn is None:
                            continue
                        ins = fn(eng)
                        k = sig[0]
                        if k[0] == 'e':
                            ins.then_inc(self.sems[ename], 1)
                        else:
                            ins.then_inc(self.dsem[k[1]][0], sig[3])
                return body
            block.tensor(mk('pe'))
            block.scalar(mk('act'))
            block.vector(mk('dve'))
            block.gpsimd(mk('pool'))
            block.sync(mk('sp'))

    def dma(self, out, in_, reads, writes, sem, eng='sp', **kw):
        self.op(eng, lambda e: e.dma_start(out=out, in_=in_, **kw), reads, writes, dma=sem)

    def cc(self, kind, groups, in_ap, out_ap, reads, writes, sem):
        self.op('pool', lambda e: e.collective_compute(kind, ALU.bypass, replica_groups=groups, ins=[in_ap], outs=[out_ap]),
                reads, writes, dma=sem, inc=1)

    def mm(self, out, lhsT, rhs, start, stop, reads, writes):
        self.op('pe', lambda e: e.matmul(out, lhsT, rhs, start=start, stop=stop), reads, writes,
                pe_acc=not start)

    def transpose(self, out, in_, ident, reads, writes):
        self.op('pe', lambda e: e.transpose(out, in_, ident), reads, writes)

    def act(self, out, in_, func, reads, writes, bias=None, scale=None, accum_out=None, eng='act'):
        kw = {}
        if bias is not None:
            kw['bias'] = bias
        if scale is not None:
            kw['scale'] = scale
        if accum_out is not None:
            kw['accum_out'] = accum_out
        self.op(eng, lambda e: e.activation(out, in_, func, **kw), reads, writes)

    def tt(self, eng, out, in0, in1, op, reads, writes):
        self.op(eng, lambda e: e.tensor_tensor(out, in0, in1, op), reads, writes)

    def ts(self, eng, out, in0, s1, s2, op0, op1, reads, writes, accum_out=None):
        if op1 is None:
            self.op(eng, lambda e: e.tensor_scalar(out, in0, s1, None, op0), reads, writes)
        elif accum_out is not None:
            self.op(eng, lambda e: e.tensor_scalar(out, in0, s1, s2, op0, op1, accum_out=accum_out), reads, writes)
        else:
            self.op(eng, lambda e: e.tensor_scalar(out, in0, s1, s2, op0, op1), reads, writes)

    def stt(self, eng, out, in0, scalar, in1, op0, op1, reads, writes):
        self.op(eng, lambda e: e.scalar_tensor_tensor(out, in0, scalar, in1, op0, op1), reads, writes)

    def copy(self, eng, out, in_, reads, writes):
        if eng == 'act':
            self.op(eng, lambda e: e.copy(out, in_), reads, writes)
        else:
            self.op(eng, lambda e: e.tensor_copy(out, in_), reads, writes)

    def memset(self, eng, ap, val, writes):
        self.op(eng, lambda e: e.memset(ap, val), (), writes)


D = 2048
EPS = 1e-6


class Ctx:
    def __init__(self):
        self.nc = bass.Bass("TRN2", target_bir_lowering=False)
        self.es = contextlib.ExitStack()
        self.S = Sched(self.nc, self.es)
        self.ps = [self.es.enter_context(self.nc.psum_tensor(f"ps{i}", [128, 512], F32)) for i in range(8)]
        self.uid = 0

    def din(self, name, shape, dt):
        return self.nc.dram_tensor(name, list(shape), dt, kind="ExternalInput").ap()

    def dout(self, name, shape, dt):
        return self.nc.dram_tensor(name, list(shape), dt, kind="ExternalOutput").ap()

    def sb(self, name, shape, dt, es=None):
        self.uid += 1
        return (es or self.es).enter_context(self.nc.sbuf_tensor(f"{name}_u{self.uid}", list(shape), dt))

    def consts(self):
        S = self.S
        self.ones32 = self.sb("ones32", [128, 128], F32)
        self.ident32 = self.sb("ident32", [128, 128], F32)
        self.ident16 = self.sb("ident16", [128, 128], BF16)
        S.memset('pool', self.ones32[:], 1.0, ['ones32'])
        S.op('pool', lambda e: e.affine_select(self.ident32[:], self.ones32[:], [[-1, 128]], ALU.is_equal, 0.0,
                                                base=0, channel_multiplier=1), ['ones32'], ['ident32'])
        S.op('pool', lambda e: e.affine_select(self.ident16[:], self.ones32[:], [[-1, 128]], ALU.is_equal, 0.0,
                                                base=0, channel_multiplier=1), ['ones32'], ['ident16'])

    def mask(self, name, cmp, base, dt=F32, pmul=1, fmul=-1, es=None):
        m = self.sb(name, [128, 128], dt, es)
        self.S.op('pool', lambda e: e.affine_select(m[:], self.ones32[:], [[fmul, 128]], cmp, 0.0, base=base,
                                                    channel_multiplier=pmul), ['ones32'], [name])
        return m

    def finish(self, out_res):
        S = self.S
        S.barrier()
        S.op('sp', lambda e: e.nop(), [], [])
        S.emit()
        self.es.close()
        return self.nc


def norm_tile(cx, x_ap, xres, wmod, shb, out_ap, outres, tag, tmp32=None):
    S = cx.S
    junk, ssq, rstd = cx.nt_junk, cx.nt_ssq, cx.nt_rstd
    S.act(junk[:], x_ap, AF.Square, xres, ['nt_junk', 'nt_ssq'], accum_out=ssq[:])
    S.ts('dve', rstd[:], ssq[:], 1.0 / D, EPS, ALU.mult, ALU.add, ['nt_ssq'], ['nt_rstd'])
    S.act(rstd[:], rstd[:], AF.Sqrt, ['nt_rstd'], ['nt_rstd'])
    S.op('dve', lambda e: e.reciprocal(rstd[:], rstd[:]), ['nt_rstd'], ['nt_rstd'])
    if shb is None:
        S.stt('dve', out_ap, x_ap, rstd[:], wmod[0][:], ALU.mult, ALU.mult, xres + ['nt_rstd', wmod[1]], outres)
    else:
        t = tmp32 if tmp32 is not None else junk
        tr = ['nt_junk'] if tmp32 is None else [tag + '_tmp']
        S.stt('dve', t[:], x_ap, rstd[:], wmod[0][:], ALU.mult, ALU.mult, xres + ['nt_rstd', wmod[1]], tr)
        S.tt('pool', out_ap, t[:], shb[0][:], ALU.add, tr + [shb[1]], outres)


def norm_alloc(cx):
    cx.nt_junk = cx.sb("nt_junk", [128, 2048], F32)
    cx.nt_ssq = cx.sb("nt_ssq", [128, 1], F32)
    cx.nt_rstd = cx.sb("nt_rstd", [128, 1], F32)


def load_row(cx, name, src_row_ap, es=None):
    t = cx.sb(name, [128, 2048], F32, es)
    cx.S.dma(t[:], src_row_ap.partition_broadcast(128), (), [name], 'row_' + name)
    return t


def make_wmod(cx, name, scb_name, sc_row, nw_row, es=None):
    scb = load_row(cx, scb_name, sc_row, es)
    nwb = load_row(cx, name, nw_row, es)
    cx.S.stt('dve', nwb[:], scb[:], 1.0, nwb[:], ALU.add, ALU.mult, [scb_name, name], [name])
    return nwb


def build_ada():
    cx = Ctx()
    S = cx.S
    cT = cx.din("cT", [128, 16, 2], F32)
    aw = cx.din("aw", [2, 2048, 1536], F32)
    ab = cx.din("ab", [2, 1536], F32)
    mod = cx.dout("mod", [2, 2, 1536], F32)
    cs = cx.sb("cs", [128, 16, 2], F32)
    S.dma(cs[:], cT, (), ['cs'], 'cs')
    S.act(cs[:], cs[:], AF.Silu, ['cs'], ['cs'])
    wsl = [cx.sb(f"aw{i}", [128, 16, 512], F32) for i in range(2)]
    bsb = cx.sb("bsb", [2, 2, 1536], F32)
    for l in range(2):
        S.dma(bsb[:, l, :], ab[l, :].partition_broadcast(2), (), [f'bsb{l}'], f'bsb{l}')
    osb = cx.sb("osb", [2, 2, 1536], F32)
    it = 0
    for l in range(2):
        for blk in range(3):
            w = wsl[it % 2]
            wn = f"aw{it % 2}"
            S.dma(w[:], aw[l, :, blk * 512:(blk + 1) * 512].rearrange("(c p) n -> p c n", p=128), (), [wn], wn)
            ps = cx.ps[it % 2]
            pn = f"ps{it % 2}"
            for k in range(16):
                S.mm(ps[0:2, :], cs[:, k, :], w[:, k, :], k == 0, k == 15, ['cs', wn], [pn])
            S.tt('dve', osb[:, l, blk * 512:(blk + 1) * 512], ps[0:2, :], bsb[:, l, blk * 512:(blk + 1) * 512], ALU.add,
                 [pn, f'bsb{l}'], ['osb'])
            it += 1
    S.dma(mod.rearrange("l b n -> b l n"), osb[:], ['osb'], ['mod'], 'out')
    return cx.finish(['mod'])


def emit_transposes_bf16(cx, src_tile, src_res, dst, dst_res, j, bank0=0, perm=False):
    S = cx.S
    for half in range(2):
        ps = cx.ps[bank0 + half]
        pn = f"ps{bank0 + half}"
        pb = ps[:].bitcast(BF16)
        for cc in range(8):
            c = half * 8 + cc
            srcv = src_tile[:, c:2048:16] if perm else src_tile[:, c * 128:(c + 1) * 128]
            S.transpose(pb[:, cc * 128:(cc + 1) * 128], srcv, cx.ident16[:],
                        src_res + ['ident16'], [pn])
        eng = 'act' if half == 0 else 'dve'
        S.copy(eng, dst[:, half * 8:(half + 1) * 8, j * 128:(j + 1) * 128],
               pb.rearrange("p (c t) -> p c t", t=128), [pn], dst_res)


def build_t0():
    cx = Ctx()
    S = cx.S
    x = cx.din("x", [1024, 2048], F32)
    rows = cx.din("rows", [3, 2048], F32)
    hT = cx.dout("hT", [2048, 1024], BF16)
    cx.consts()
    norm_alloc(cx)
    wmod = make_wmod(cx, "wmod", "scb", rows[1, :], rows[2, :])
    shb = load_row(cx, "shb", rows[0, :])
    xs = [cx.sb(f"xt{i}", [128, 2048], F32) for i in range(2)]
    hb = [cx.sb(f"hb{i}", [128, 2048], BF16) for i in range(2)]
    hTb = cx.sb("hTb", [128, 16, 1024], BF16)
    for j in range(8):
        xt, xn = xs[j % 2], f"xt{j % 2}"
        S.dma(xt[:], x[j * 128:(j + 1) * 128, :], (), [xn], xn)
        norm_tile(cx, xt[:], [xn], (wmod, 'wmod'), (shb, 'shb'), hb[j % 2][:], [f"hb{j % 2}"], 't0')
        emit_transposes_bf16(cx, hb[j % 2], [f"hb{j % 2}"], hTb, ['hTb'], j, bank0=(j % 2) * 2)
    S.dma(hT.rearrange("(c p) t -> p c t", p=128), hTb[:], ['hTb'], ['hT'], 'out')
    return cx.finish(['hT'])


NE = 32
DEBUG_C = False
CAP = 128
BIG = 30000.0


def build_c(K, last):
    KC = K // 128
    cx = Ctx()
    S = cx.S
    nc = cx.nc
    x_in = cx.din("x", [1024, 2048], F32)
    mixT = cx.din("mixT", [K, 1024], BF16)
    wout = cx.din("wout", [K, 2048], F32)
    rows = cx.din("rows", [8, 2048], F32)
    wr = cx.din("wr", [2048, 36], F32)
    w1 = cx.din("w1", [NE, 2048, 512], F32)
    w3 = cx.din("w3", [NE, 2048, 512], F32)
    w2 = cx.din("w2", [NE, 512, 2048], F32)
    if last:
        xo = cx.dout("out", [1024, 2048], F32)
    else:
        xo = cx.dout("xo", [1024, 2048], F32)
        hTn = cx.dout("hTn", [2048, 1024], BF16)
    cx.consts()
    xs = cx.sb("xs", [128, 8, 2048], F32)
    S.dma(xs[:], x_in.rearrange("(j p) d -> p j d", p=128), (), ['xs'], 'xs')
    XR = [f'xs{j}' for j in range(8)]
    for j in range(8):
        S.lastw[XR[j]] = S.lastw['xs']

    with contextlib.ExitStack() as es:
        mT = cx.sb("mT", [128, KC, 1024], BF16, es)
        S.dma(mT[:], mixT.rearrange("(c p) t -> p c t", p=128), (), ['mT'], 'mT')
        g1b = load_row(cx, "g1b", rows[0, :], es)
        wsl = [cx.sb(f"wo{i}", [128, KC, 512], BF16, es) for i in range(2)]
        tmp = [cx.sb(f"optmp{i}", [128, 512], F32, es) for i in range(2)]
        it = 0
        for n in range(4):
            w, wn = wsl[n % 2], f"wo{n % 2}"
            S.dma(w[:], wout[:, n * 512:(n + 1) * 512].rearrange("(c p) n -> p c n", p=128), (), [wn], wn, eng='pool')
            for j in range(8):
                ps, pn = cx.ps[it % 4], f"ps{it % 4}"
                for k in range(KC):
                    S.mm(ps[:], mT[:, k, j * 128:(j + 1) * 128], w[:, k, :], k == 0, k == KC - 1, ['mT', wn], [pn])
                t, tn = tmp[it % 2], f"optmp{it % 2}"
                S.tt('dve', t[:], ps[:], g1b[:, n * 512:(n + 1) * 512], ALU.mult, [pn, 'g1b'], [tn])
                S.tt('pool', xs[:, j, n * 512:(n + 1) * 512], xs[:, j, n * 512:(n + 1) * 512], t[:], ALU.add,
                     [tn, XR[j]], [XR[j]])
                it += 1
    S.barrier()

    hTb = cx.sb("hTbm", [128, 16, 1024], BF16)
    lg = cx.sb("lg", [128, 8, 36], F32)
    with contextlib.ExitStack() as es:
        norm_alloc_es(cx, es)
        wmod2 = make_wmod(cx, "wmod2", "sc2b", rows[2, :], rows[4, :], es)
        sh2b = load_row(cx, "sh2b", rows[1, :], es)
        wrs = cx.sb("wrs", [128, 16, 36], F32, es)
        S.dma(wrs[:], wr.rearrange("(c p) n -> p c n", p=128), (), ['wrs'], 'wrs')
        h32 = [cx.sb(f"h32_{i}", [128, 2048], F32, es) for i in range(2)]
        hbt = [cx.sb(f"hbt_{i}", [128, 2048], BF16, es) for i in range(2)]
        hT32 = [cx.sb(f"hT32_{i}", [128, 16, 128], F32, es) for i in range(2)]
        for j in range(8):
            h, hn = h32[j % 2], f"h32_{j % 2}"
            norm_tile(cx, xs[:, j, :], [XR[j]], (wmod2, 'wmod2'), (sh2b, 'sh2b'), h[:], [hn], 'n2')
            S.copy('act', hbt[j % 2][:], h[:], [hn], [f'hbt{j % 2}'])
            emit_transposes_bf16(cx, hbt[j % 2], [f'hbt{j % 2}'], hTb, ['hTbm'], j, bank0=6, perm=True)
            hT, hTn_ = hT32[j % 2], f"hT32_{j % 2}"
            for q in range(4):
                ps, pn = cx.ps[q], f"ps{q}"
                for cc in range(4):
                    c = q * 4 + cc
                    S.transpose(ps[:, cc * 128:(cc + 1) * 128], h[:, c * 128:(c + 1) * 128], cx.ident32[:],
                                [hn, 'ident32'], [pn])
                S.copy('dve' if q % 2 else 'act', hT[:, q * 4:(q + 1) * 4, :], ps[:].rearrange("p (c t) -> p c t", t=128),
                       [pn], [hTn_])
            pl, pln = cx.ps[4 + (j % 2)], f"ps{4 + (j % 2)}"
            for c in range(16):
                S.mm(pl[:, 0:36], hT[:, c, :], wrs[:, c, :], c == 0, c == 15, [hTn_, 'wrs'], [pln])
            S.copy('dve', lg[:, j, :], pl[:, 0:36], [pln], ['lg'])
    S.barrier()

    Wt = cx.sb("Wt", [128, 8, 32], F32)
    with contextlib.ExitStack() as es:
        def t(name, shape, dt=F32):
            return cx.sb(name, shape, dt, es)
        R = ['rt']
        gl = lg[:, :, 0:4]
        el = lg[:, :, 4:36]
        gmax = t("gmax", [128, 8]); gsh = t("gsh", [128, 8, 4]); gsum = t("gsum", [128, 8]); gw = t("gw", [128, 8])
        goh = t("goh", [128, 8, 4]); pen = t("pen", [128, 8, 4]); elm = t("elm", [128, 8, 32])
        m1 = t("m1", [128, 8]); is1 = t("is1", [128, 8, 32]); m2 = t("m2", [128, 8]); is2 = t("is2", [128, 8, 32])
        dd = t("dd", [128, 8]); p1 = t("p1", [128, 8]); p2 = t("p2", [128, 8]); tmpa = t("tmpa", [128, 8, 32])
        A = t("A", [128, 8, 32]); A16 = t("A16", [128, 8, 32], BF16)
        def V(fn):
            S.op('dve', fn, ['lg'] + R, R)
        def bc(ap, n):
            return ap.unsqueeze(2).broadcast_to([128, 8, n])
        V(lambda e: e.tensor_reduce(gmax[:], gl, AX.X, ALU.max))
        V(lambda e: e.tensor_tensor(gsh[:], gl, bc(gmax[:], 4), ALU.subtract))
        V(lambda e: e.tensor_tensor(goh[:], gl, bc(gmax[:], 4), ALU.is_equal))
        S.act(gsh[:], gsh[:], AF.Exp, R, R)
        V(lambda e: e.tensor_reduce(gsum[:], gsh[:], AX.X, ALU.add))
        V(lambda e: e.reciprocal(gw[:], gsum[:]))
        V(lambda e: e.tensor_scalar(pen[:], goh[:], BIG, -BIG, ALU.mult, ALU.add))
        V(lambda e: e.tensor_tensor(elm[:].rearrange("p j (g k) -> p j g k", k=8), el.rearrange("p j (g k) -> p j g k", k=8),
                                    pen[:].unsqueeze(3).broadcast_to([128, 8, 4, 8]), ALU.add))
        V(lambda e: e.tensor_reduce(m1[:], elm[:], AX.X, ALU.max))
        V(lambda e: e.tensor_tensor(is1[:], elm[:], bc(m1[:], 32), ALU.is_equal))
        V(lambda e: e.scalar_tensor_tensor(elm[:], is1[:], -BIG, elm[:], ALU.mult, ALU.add))
        V(lambda e: e.tensor_reduce(m2[:], elm[:], AX.X, ALU.max))
        V(lambda e: e.tensor_tensor(is2[:], elm[:], bc(m2[:], 32), ALU.is_equal))
        V(lambda e: e.tensor_tensor(dd[:], m2[:], m1[:], ALU.subtract))
        S.act(dd[:], dd[:], AF.Exp, R, R)
        V(lambda e: e.tensor_scalar(p1[:], dd[:], 1.0, None, ALU.add))
        V(lambda e: e.reciprocal(p1[:], p1[:]))
        V(lambda e: e.tensor_tensor(p2[:], dd[:], p1[:], ALU.mult))
        V(lambda e: e.tensor_tensor(p1[:], p1[:], gw[:], ALU.mult))
        V(lambda e: e.tensor_tensor(p2[:], p2[:], gw[:], ALU.mult))
        V(lambda e: e.tensor_tensor(Wt[:], is1[:], bc(p1[:], 32), ALU.mult))
        V(lambda e: e.tensor_tensor(tmpa[:], is2[:], bc(p2[:], 32), ALU.mult))
        V(lambda e: e.tensor_tensor(Wt[:], Wt[:], tmpa[:], ALU.add))
        V(lambda e: e.tensor_tensor(A[:], is1[:], is2[:], ALU.add))
        V(lambda e: e.tensor_copy(A16[:], A[:]))
    if DEBUG_C:
        dlg = cx.dout("dlg", [128, 8, 36], F32)
        dWt = cx.dout("dWt", [128, 8, 32], F32)
        S.dma(dlg, lg[:], ['lg'], ['dlg'], 'dlg')
        S.dma(dWt, Wt[:], ['rt'], ['dWt'], 'dWt')
    S.barrier()

    with contextlib.ExitStack() as es:
        g2b = load_row(cx, "g2b", rows[3, :], es)
        NW = 5
        wslot = [cx.sb(f"wsl{i}", [128, 8192], BF16, es) for i in range(NW)]
        s1 = [cx.sb(f"s1_{i}", [128, 512], F32, es) for i in range(2)]
        actT = cx.sb("actT", [128, 4, 1024], BF16, es)
        ytmp = [cx.sb(f"ytmp{i}", [128, 512], F32, es) for i in range(2)]
        widx = [0]

        def wload(kind, e):
            src = {'w1': w1, 'w3': w3, 'w2': w2}[kind]
            i = widx[0] % NW
            widx[0] += 1
            wn = f"wsl{i}"
            nn = 2048 if kind == 'w2' else 512
            dst = wslot[i][:].rearrange("p (c n) -> p c n", n=nn)
            if kind == 'w2':
                S.dma(dst, src[e].rearrange("(c p) n -> p c n", p=128), (), [wn], wn, eng='pool')
            else:
                S.dma(dst, src[e].rearrange("(p c) n -> p c n", c=16), (), [wn], wn, eng='pool')
            return (wslot[i], wn)

        cur = [wload('w1', 0), wload('w3', 0), wload('w2', 0)]
        it = 0
        for e_ in range(NE):
            (w1s, w1n), (w3s, w3n), (w2s, w2n) = cur
            nxt = [None, None, None]
            if e_ + 1 < NE:
                nxt[0] = wload('w1', e_ + 1)
                nxt[1] = wload('w3', e_ + 1)
            w1v = w1s[:].rearrange("p (c n) -> p c n", n=512)
            w3v = w3s[:].rearrange("p (c n) -> p c n", n=512)
            w2v = w2s[:].rearrange("p (c n) -> p c n", n=2048)
            for th in range(2):
                for fb in range(4):
                    pA, pan = cx.ps[(it % 2) * 2], f"ps{(it % 2) * 2}"
                    pB, pbn = cx.ps[(it % 2) * 2 + 1], f"ps{(it % 2) * 2 + 1}"
                    for k in range(16):
                        S.mm(pA[:], w1v[:, k, fb * 128:(fb + 1) * 128], hTb[:, k, th * 512:(th + 1) * 512], k == 0, k == 15,
                             [w1n, 'hTbm'], [pan])
                    for k in range(16):
                        S.mm(pB[:], w3v[:, k, fb * 128:(fb + 1) * 128], hTb[:, k, th * 512:(th + 1) * 512], k == 0, k == 15,
                             [w3n, 'hTbm'], [pbn])
                    ss, sn = s1[it % 2], f"s1_{it % 2}"
                    S.act(ss[:], pA[:], AF.Silu, [pan], [sn])
                    S.tt('dve', actT[:, fb, th * 512:(th + 1) * 512], ss[:], pB[:], ALU.mult, [sn, pbn], [f'actT{th}'])
                    it += 1
            if e_ + 1 < NE:
                nxt[2] = wload('w2', e_ + 1)
            for j in range(8):
                for n in range(4):
                    pY, pyn = cx.ps[4 + (it % 4)], f"ps{4 + (it % 4)}"
                    for fb in range(4):
                        S.mm(pY[:], actT[:, fb, j * 128:(j + 1) * 128], w2v[:, fb, n * 512:(n + 1) * 512], fb == 0, fb == 3,
                             [f'actT{j // 4}', w2n], [pyn])
                    yt, ytn = ytmp[it % 2], f"ytmp{it % 2}"
                    S.stt('dve', yt[:], pY[:], Wt[:, j, e_:e_ + 1], g2b[:, n * 512:(n + 1) * 512], ALU.mult, ALU.mult,
                          [pyn, 'g2b'], [ytn])
                    S.tt('pool', xs[:, j, n * 512:(n + 1) * 512], xs[:, j, n * 512:(n + 1) * 512], yt[:], ALU.add,
                         [ytn, XR[j]], [XR[j]])
                    it += 1
            cur = nxt
    S.barrier()

    with contextlib.ExitStack() as es:
        norm_alloc_es(cx, es)
        if last:
            nwb = load_row(cx, "nwN", rows[7, :], es)
            ob = [cx.sb(f"ob{i}", [128, 2048], F32, es) for i in range(2)]
            for j in range(8):
                o, on = ob[j % 2], f"ob{j % 2}"
                norm_tile(cx, xs[:, j, :], [XR[j]], (nwb, 'nwN'), None, o[:], [on], 'nf')
                S.dma(xo[j * 128:(j + 1) * 128, :], o[:], [on], ['xo'], f'outd{j % 2}')
        else:
            S.dma(xo.rearrange("(j p) d -> p j d", p=128), xs[:], XR, ['xo'], 'outx')
            wmodN = make_wmod(cx, "wmodN", "scNb", rows[6, :], rows[7, :], es)
            shNb = load_row(cx, "shNb", rows[5, :], es)
            hbn = [cx.sb(f"hbn{i}", [128, 2048], BF16, es) for i in range(2)]
            hTb = cx.sb("hTb", [128, 16, 1024], BF16, es)
            for j in range(8):
                norm_tile(cx, xs[:, j, :], [XR[j]], (wmodN, 'wmodN'), (shNb, 'shNb'), hbn[j % 2][:], [f"hbn{j % 2}"], 'nn')
                emit_transposes_bf16(cx, hbn[j % 2], [f"hbn{j % 2}"], hTb, ['hTb'], j, bank0=(j % 2) * 2)
            S.dma(hTn.rearrange("(c p) t -> p c t", p=128), hTb[:], ['hTb'], ['hTn'], 'outh')
    return cx.finish(['xo'] + ([] if last else ['hTn']) + (['dlg', 'dWt'] if DEBUG_C else []))


def norm_alloc_es(cx, es):
    cx.nt_junk = cx.sb("nt_junk", [128, 2048], F32, es)
    cx.nt_ssq = cx.sb("nt_ssq", [128, 1], F32, es)
    cx.nt_rstd = cx.sb("nt_rstd", [128, 1], F32, es)


def build_ssd():
    cx = Ctx()
    S = cx.S
    hT = cx.din("hT", [2048, 8192], BF16)
    Wg = cx.din("Wg", [2048, 1296], F32)
    cwd = cx.din("cw", [768, 5], F32)
    cbd = cx.din("cb", [768, 1], F32)
    hp = cx.din("hp", [3, 16], F32)
    nwd = cx.din("nw", [1, 512], F32)
    ynT = cx.dout("ynT", [512, 8192], BF16)
    hTv = hT.rearrange("(c p) t -> p c t", p=128)
    ynTv = ynT.rearrange("(k p) t -> p k t", p=128)
    cx.consts()
    m_le = cx.mask("m_le", ALU.is_ge, 0, pmul=-1, fmul=1)
    m_gt = cx.mask("m_gt", ALU.is_gt, 0, pmul=1, fmul=-1)
    m_ge = cx.mask("m_ge", ALU.is_ge, 0, pmul=1, fmul=-1)
    m_lt = cx.mask("m_lt", ALU.is_gt, 0, pmul=-1, fmul=1)
    ones = cx.ones32
    sb = cx.sb
    WZ = sb("WZ", [128, 16, 512], BF16)
    Wgv = Wg.rearrange("(c p) n -> p c n", p=128)
    S.dma(WZ[:], Wgv[:, :, 0:512], (), ['WZ'], 'WZ', eng='pool')
    cw = sb("cw", [128, 6, 5], F32)
    cb = sb("cb", [128, 6, 1], F32)
    S.dma(cw[:], cwd.rearrange("(b p) j -> p b j", p=128), (), ['cw'], 'cw')
    S.dma(cb[:], cbd.rearrange("(b p) j -> p b j", p=128), (), ['cb'], 'cb', allow_slow_non_contiguous=True)
    dtb = sb("dtb", [128, 16], F32)
    a_b = sb("a_b", [128, 16], F32)
    Db = sb("Db", [128, 16], F32)
    nwb = sb("nwb", [128, 512], F32)
    S.dma(dtb[:], hp[0, :].partition_broadcast(128), (), ['dtb'], 'dtb')
    S.dma(a_b[:], hp[1, :].partition_broadcast(128), (), ['a_b'], 'a_b')
    S.dma(Db[:], hp[2, :].partition_broadcast(128), (), ['Db'], 'Db')
    S.dma(nwb[:], nwd[0, :].partition_broadcast(128), (), ['nwb'], 'nwb')
    S.act(a_b[:], a_b[:], AF.Exp, ['a_b'], ['a_b'])
    S.ts('dve', a_b[:], a_b[:], -1.0, None, ALU.mult, None, ['a_b'], ['a_b'])

    xs_tok = sb("xs_tok", [128, 32, 512], BF16)
    B_tok = sb("B_tok", [128, 32, 128], BF16)
    BT = sb("BT", [128, 4096], BF16)
    CT = sb("CT", [128, 4096], BF16)
    dt = sb("dt", [128, 32, 16], F32)
    adt = sb("adt", [128, 32, 16], F32)
    ecs = sb("ecs", [128, 32, 16], F32)
    edec = sb("edec", [128, 32, 16], F32)
    etot = sb("etot", [128, 32, 16], F32)
    Sin_f = sb("Sin_f", [128, 32, 512], BF16)
    Sf = sb("Sf", [128, 512], F32)
    Sb = sb("Sb", [128, 512], F32)
    Sb16 = sb("Sb16", [128, 512], BF16)
    dd = sb("dd", [128, 8], F32)
    xdtd = sb("xdtd", [128, 512], BF16)
    P = cx.ps

    def b64(ap):
        return ap.unsqueeze(2).broadcast_to([128, 8, 64])

    def h64(ap):
        return ap.rearrange("p (h d) -> p h d", d=64)

    for b in range(2):
        tb = b * 4096
        with contextlib.ExitStack() as es:
            WA = sb("WA", [128, 16, 784], BF16, es)
            S.dma(WA[:], Wgv[:, :, 512:1296], (), ['WA'], 'WA', eng='pool')
            hwin = [sb(f"hwin{i}", [128, 16, 516], BF16, es) for i in range(2)]
            xb4 = sb("xb4", [128, 4, 16], F32, es)
            dd4 = sb("dd4", [128, 4, 8], F32, es)
            xdtd2 = [sb(f"xdtd2_{i}", [128, 512], BF16, es) for i in range(2)]
            u = sb("u", [128, 6, 516], F32, es)
            acc = [sb(f"acc{i}", [128, 512], F32, es) for i in range(2)]
            xsT = [sb(f"xsT{i}", [128, 512], BF16, es) for i in range(2)]
            xb_ = sb("xb_", [128, 16], F32, es)
            S.memset('dve', Sf[:], 0.0, ['Sf'])
            def part_m(sc):
                hw, hwn = hwin[sc % 2], f"hwin{sc % 2}"
                lo, hi = (2 if sc == 0 else 0), (514 if sc == 7 else 516)
                if sc == 0:
                    S.memset('pool', hw[:, :, 0:2], 0.0, [hwn])
                if sc == 7:
                    S.memset('pool', hw[:, :, 514:516], 0.0, [hwn])
                S.dma(hw[:, :, lo:hi], hTv[:, :, tb + sc * 512 - 2 + lo: tb + sc * 512 - 2 + hi], (), [hwn], hwn)
                for blk in range(6):
                    ps, pn = P[blk % 2], f"ps{blk % 2}"
                    for k in range(16):
                        S.mm(ps[:], WA[:, k, blk * 128:(blk + 1) * 128], hw[:, k, 2:514], k == 0, k == 15, ['WA', hwn], [pn])
                    S.copy('act', u[:, blk, 2:514], ps[:], [pn], ['u'])
                    for side in range(2):
                        cols = slice(0, 2) if side == 0 else slice(514, 516)
                        for k in range(16):
                            S.mm(P[2][:, blk * 4 + side * 2: blk * 4 + side * 2 + 2], WA[:, k, blk * 128:(blk + 1) * 128],
                                 hw[:, k, cols], k == 0, k == 15, ['WA', hwn], ['ps2'])
                pv = P[2][:, 0:24].rearrange("p (b f) -> p b f", f=4)
                S.copy('dve', u[:, :, 0:2], pv[:, :, 0:2], ['ps2'], ['u'])
                S.copy('dve', u[:, :, 514:516], pv[:, :, 2:4], ['ps2'], ['u'])
            def part_c(sc):
                hw, hwn = hwin[sc % 2], f"hwin{sc % 2}"
                for blk in range(6):
                    ce = 'dve'
                    a, an = acc[blk % 2], f"acc{blk % 2}"
                    S.ts(ce, a[:], u[:, blk, 0:512], cw[:, blk, 0:1], cb[:, blk, 0:1], ALU.mult, ALU.add, ['u', 'cw', 'cb'], [an])
                    for j in range(1, 5):
                        S.stt(ce, a[:], u[:, blk, j:j + 512], cw[:, blk, j:j + 1], a[:], ALU.mult, ALU.add, ['u', 'cw', an], [an])
                    if blk == 5:
                        S.act(CT[:, sc * 512:(sc + 1) * 512], a[:], AF.Silu, [an], ['CT'])
                        continue
                    if blk == 4:
                        src = BT[:, sc * 512:(sc + 1) * 512]
                        S.act(src, a[:], AF.Silu, [an], ['BT'])
                        srcres = ['BT']
                    else:
                        src = xsT[blk % 2][:]
                        S.act(src, a[:], AF.Silu, [an], [f'xsT{blk % 2}'])
                        srcres = [f'xsT{blk % 2}']
                    pt, ptn = P[3 + blk % 2], f"ps{3 + blk % 2}"
                    ptb = pt[:].bitcast(BF16)
                    for cc in range(4):
                        S.transpose(ptb[:, cc * 128:(cc + 1) * 128], src[:, cc * 128:(cc + 1) * 128], cx.ident16[:],
                                    srcres + ['ident16'], [ptn])
                    if blk == 4:
                        S.copy('dve', B_tok[:, sc * 4:(sc + 1) * 4, :], ptb[:, 0:512].rearrange("p (c n) -> p c n", n=128),
                               [ptn], ['B_tok'])
                    else:
                        S.copy('dve', xs_tok[:, sc * 4:(sc + 1) * 4, blk * 128:(blk + 1) * 128],
                               ptb[:, 0:512].rearrange("p (c n) -> p c n", n=128), [ptn], ['xs_tok'])
            def part_d(sc):
                hw, hwn = hwin[sc % 2], f"hwin{sc % 2}"
                c0 = sc * 4
                for cc in range(4):
                    for k in range(16):
                        S.mm(P[5][:, cc * 16:(cc + 1) * 16], hw[:, k, 2 + cc * 128: 2 + (cc + 1) * 128], WA[:, k, 768:784],
                             k == 0, k == 15, ['WA', hwn], ['ps5'])
                S.tt('dve', xb4[:], P[5][:, 0:64].rearrange("p (c n) -> p c n", n=16),
                     dtb[:].unsqueeze(1).broadcast_to([128, 4, 16]), ALU.add, ['ps5', 'dtb'], ['xb4'])
                S.act(xb4[:], xb4[:], AF.Exp, ['xb4'], ['xb4'])
                S.act(dt[:, c0:c0 + 4, :], xb4[:], AF.Ln, ['xb4', 'ones32'], ['dt'], bias=ones[:, 0:1])
                S.tt('dve', adt[:, c0:c0 + 4, :], dt[:, c0:c0 + 4, :], a_b[:].unsqueeze(1).broadcast_to([128, 4, 16]), ALU.mult,
                     ['dt', 'a_b'], ['adt'])
                for mi, (m, mn) in enumerate([(m_le, 'm_le'), (m_gt, 'm_gt'), (m_ge, 'm_ge'), (m_lt, 'm_lt'), (ones, 'ones32')]):
                    S.mm(P[6][:, mi * 64:(mi + 1) * 64], m[:], adt[:, c0:c0 + 4, :].rearrange("p c n -> p (c n)"), True, True,
                         [mn, 'adt'], ['ps6'])
                P6 = P[6][:, 0:320].rearrange("p (m c n) -> p m c n", m=5, c=4)
                S.act(ecs[:, c0:c0 + 4, 0:8], P6[:, 0, :, 0:8], AF.Exp, ['ps6'], ['ecs'])
                S.act(ecs[:, c0:c0 + 4, 8:16], P6[:, 2, :, 8:16], AF.Exp, ['ps6'], ['ecs'])
                S.act(edec[:, c0:c0 + 4, 0:8], P6[:, 1, :, 0:8], AF.Exp, ['ps6'], ['edec'])
                S.act(edec[:, c0:c0 + 4, 8:16], P6[:, 3, :, 8:16], AF.Exp, ['ps6'], ['edec'])
                S.act(etot[:, c0:c0 + 4, :], P6[:, 4, :, :], AF.Exp, ['ps6'], ['etot'])
                S.tt('dve', dd4[:], dt[:, c0:c0 + 4, 0:8], edec[:, c0:c0 + 4, 0:8], ALU.mult, ['dt', 'edec'], ['dd4'])
                for cc in range(4):
                    c = c0 + cc
                    xd, xdn = xdtd2[cc % 2], f"xdtd2_{cc % 2}"
                    S.tt('pool', h64(xd[:]), h64(xs_tok[:, c, :]), b64(dd4[:, cc, :]), ALU.mult, ['xs_tok', 'dd4'], [xdn])
                    S.mm(P[7][:], B_tok[:, c, :], xd[:], True, True, ['B_tok', xdn], ['ps7'])
                    S.copy('act', Sin_f[:, c, :], Sf[:], ['Sf'], ['Sin_f'])
                    S.tt('dve', h64(Sf[:]), h64(Sf[:]), b64(etot[:, c, 0:8]), ALU.mult, ['Sf', 'etot'], ['Sf'])
                    S.tt('dve', Sf[:], Sf[:], P[7][:], ALU.add, ['Sf', 'ps7'], ['Sf'])
            part_m(0)
            part_c(0)
            for sc in range(8):
                if sc + 1 < 8:
                    part_m(sc + 1)
                part_d(sc)
                if sc + 1 < 8:
                    part_c(sc + 1)
        S.barrier()
        with contextlib.ExitStack() as es:
            def two(name, shape, dtp):
                return [sb(f"{name}{i}", shape, dtp, es) for i in range(2)]
            hz = two("hz", [128, 16, 128], BF16)
            sz = two("sz", [128, 512], F32)
            CBm = [two(f"CBm{d}_", [128, 128], F32) for d in range(2)]
            A = [two(f"A{d}_", [128, 8, 128], F32) for d in range(2)]
            E = [[sb(f"E{d}{q}", [128, 4, 128], F32, es) for q in range(2)] for d in range(2)]
            MT = [two(f"MT{d}_", [128, 8, 128], BF16) for d in range(2)]
            xdt = [two(f"xdt{d}_", [128, 512], BF16) for d in range(2)]
            y = two("y", [128, 512], F32)
            t2 = two("t2", [128, 512], F32)
            t3 = two("t3", [128, 512], F32)
            yn = two("yn", [128, 512], BF16)
            junk = two("junk", [128, 512], F32)
            ssq = two("ssq", [128, 1], F32)
            rstd = two("rstd", [128, 1], F32)
            xdb = two("xdb", [128, 512], BF16)
            ddb = two("ddb", [128, 8], F32)
            yo = two("yo", [128, 4, 512], BF16)
            S.memset('dve', Sb[:], 0.0, ['Sb'])
            S.memset('dve', Sb16[:], 0.0, ['Sb16'])

            def stage_a(c):
                p = c % 2
                hzz, hzn = hz[p], f"hz{p}"
                S.dma(hzz[:], hTv[:, :, tb + c * 128: tb + (c + 1) * 128], (), [hzn], hzn)
                csl = slice(c * 128, (c + 1) * 128)
                S.mm(P[1][:, 0:128], BT[:, csl], CT[:, csl], True, True, ['BT', 'CT'], ['ps1'])
                for d in range(2):
                    mask2, m2n = (m_le, 'm_le') if d == 0 else (m_ge, 'm_ge')
                    S.tt('pool', A[d][p][:], mask2[:].unsqueeze(1).broadcast_to([128, 8, 128]),
                         adt[:, c, d * 8:(d + 1) * 8].unsqueeze(2).broadcast_to([128, 8, 128]), ALU.mult, [m2n, 'adt'], [f'A{d}{p}'])
                S.tt('dve', CBm[0][p][:], P[1][:, 0:128], m_le[:], ALU.mult, ['ps1', 'm_le'], [f'CBm0{p}'])
                S.tt('dve', CBm[1][p][:], P[1][:, 0:128], m_ge[:], ALU.mult, ['ps1', 'm_ge'], [f'CBm1{p}'])
                for d in range(2):
                    S.tt('dve', h64(xdt[d][p][:]), h64(xs_tok[:, c, :]), b64(dt[:, c, d * 8:(d + 1) * 8]), ALU.mult,
                         ['xs_tok', 'dt'], [f'xdt{d}{p}'])
                S.tt('dve', ddb[p][:], dt[:, c, 8:16], edec[:, c, 8:16], ALU.mult, ['dt', 'edec'], [f'ddb{p}'])
                S.tt('pool', h64(xdb[p][:]), h64(xs_tok[:, c, :]), b64(ddb[p][:]), ALU.mult, ['xs_tok', f'ddb{p}'], [f'xdb{p}'])
                S.tt('pool', h64(t3[p][:]), h64(xs_tok[:, c, :]), b64(Db[:, 0:8]), ALU.mult, ['xs_tok', 'Db'], [f't3{p}'])
                for q in range(2):
                    for d in range(2):
                        Lm, lmn = (m_gt, 'm_gt') if d == 0 else (m_lt, 'm_lt')
                        pd, pdn = P[2 + d], f"ps{2 + d}"
                        S.mm(pd[:], Lm[:], A[d][p][:, 4 * q:4 * q + 4, :].rearrange("p h l -> p (h l)"), True, True,
                             [lmn, f'A{d}{p}'], [pdn])
                        S.act(E[d][q][:].rearrange("p h l -> p (h l)"), pd[:], AF.Exp, [pdn], [f'E{d}{q}'])
                    for d in range(2):
                        S.tt('pool', MT[d][p][:, 4 * q:4 * q + 4, :], E[d][q][:],
                             CBm[d][p][:].unsqueeze(1).broadcast_to([128, 4, 128]), ALU.mult, [f'E{d}{q}', f'CBm{d}{p}'],
                             [f'MT{d}{p}'])
                for k in range(16):
                    S.mm(P[0][:], hzz[:, k, :], WZ[:, k, :], k == 0, k == 15, [hzn, 'WZ'], ['ps0'])
                S.act(sz[p][:], P[0][:], AF.Silu, ['ps0'], [f'sz{p}'])

            def stage_b(c):
                p = c % 2
                csl = slice(c * 128, (c + 1) * 128)
                for h in range(8):
                    hs = slice(h * 64, (h + 1) * 64)
                    S.mm(P[4][:, hs], MT[0][p][:, h, :], xdt[0][p][:, hs], True, False, [f'MT0{p}', f'xdt0{p}'], ['ps4'])
                    S.mm(P[4][:, hs], MT[1][p][:, h, :], xdt[1][p][:, hs], False, True, [f'MT1{p}', f'xdt1{p}'], ['ps4'])
                S.mm(P[5][:], CT[:, csl], Sin_f[:, c, :], True, True, ['CT', 'Sin_f'], ['ps5'])
                S.mm(P[6][:], CT[:, csl], Sb16[:], True, True, ['CT', 'Sb16'], ['ps6'])
                S.mm(P[7][:], B_tok[:, c, :], xdb[p][:], True, True, ['B_tok', f'xdb{p}'], ['ps7'])
                S.tt('dve', h64(Sb[:]), h64(Sb[:]), b64(etot[:, c, 8:16]), ALU.mult, ['Sb', 'etot'], ['Sb'])
                S.tt('dve', Sb[:], Sb[:], P[7][:], ALU.add, ['Sb', 'ps7'], ['Sb'])
                S.copy('act', Sb16[:], Sb[:], ['Sb'], ['Sb16'])
                yy, yyn = y[p], f'y{p}'
                S.tt('dve', h64(yy[:]), h64(P[5][:]), b64(ecs[:, c, 0:8]), ALU.mult, ['ps5', 'ecs'], [yyn])
                S.tt('dve', h64(t2[p][:]), h64(P[6][:]), b64(ecs[:, c, 8:16]), ALU.mult, ['ps6', 'ecs'], [f't2{p}'])
                S.tt('pool', t2[p][:], t2[p][:], t3[p][:], ALU.add, [f't2{p}', f't3{p}'], [f't2{p}'])
                S.tt('dve', yy[:], yy[:], P[4][:], ALU.add, [yyn, 'ps4'], [yyn])
                S.tt('pool', yy[:], yy[:], t2[p][:], ALU.add, [yyn, f't2{p}'], [yyn])
                S.tt('pool', yy[:], yy[:], sz[p][:], ALU.mult, [yyn, f'sz{p}'], [yyn])
                S.act(junk[p][:], yy[:], AF.Square, [yyn], [f'junk{p}', f'ssq{p}'], accum_out=ssq[p][:])
                S.ts('dve', rstd[p][:], ssq[p][:], 1.0 / 512, EPS, ALU.mult, ALU.add, [f'ssq{p}'], [f'rstd{p}'])
                S.act(rstd[p][:], rstd[p][:], AF.Sqrt, [f'rstd{p}'], [f'rstd{p}'])
                S.op('dve', lambda e, p=p: e.reciprocal(rstd[p][:], rstd[p][:]), [f'rstd{p}'], [f'rstd{p}'])
                S.stt('dve', yn[p][:], yy[:], rstd[p][:], nwb[:], ALU.mult, ALU.mult, [yyn, f'rstd{p}', 'nwb'], [f'yn{p}'])
                ptb = P[7][:].bitcast(BF16)
                for blk in range(4):
                    S.transpose(ptb[:, blk * 128:(blk + 1) * 128], yn[p][:, blk * 128:(blk + 1) * 128], cx.ident16[:],
                                [f'yn{p}', 'ident16'], ['ps7'])
                sc = c // 4
                yoo, yon = yo[sc % 2], f"yo{sc % 2}"
                S.copy('act', yoo[:, :, (c % 4) * 128:(c % 4 + 1) * 128], ptb[:, 0:512].rearrange("p (k t) -> p k t", t=128),
                       ['ps7'], [yon])
                if c % 4 == 0:
                    S.dma(ynTv[:, :, tb + sc * 512: tb + (sc + 1) * 512], yoo[:], [yon], ['ynT'], f'o{yon}')

            stage_a(31)
            for c in range(31, -1, -1):
                if c > 0:
                    stage_a(c - 1)
                stage_b(c)
        S.barrier()
    return cx.finish(['ynT'])


def build_na():
    cx = Ctx()
    S = cx.S
    sb = cx.sb
    P = cx.ps
    hT = cx.din("hT", [2048, 8192], BF16)
    Wqd = cx.din("Wq", [2048, 768], F32)
    biasd = cx.din("bias", [2, 15, 128, 64], F32)
    oT = cx.dout("oT", [256, 8192], BF16)
    hTv = hT.rearrange("(c p) t -> p c t", p=128)
    oTv = oT.rearrange("(k p) t -> p k t", p=128)
    cx.consts()
    QT = sb("QT", [128, 2, 8192], BF16)
    KT = sb("KT", [128, 2, 8192], BF16)
    V64 = sb("V64", [64, 128, 256], BF16)
    NBIG = 30000.0
    with contextlib.ExitStack() as es:
        Wq = sb("Wq", [128, 16, 768], BF16, es)
        S.dma(Wq[:], Wqd.rearrange("(c p) n -> p c n", p=128), (), ['Wq'], 'Wq', eng='pool')
        hwl = [sb(f"hw{i}", [128, 16, 512], BF16, es) for i in range(2)]
        it = 0
        for blk in range(16):
            hw, hwn = hwl[blk % 2], f"hw{blk % 2}"
            S.dma(hw[:], hTv[:, :, blk * 512:(blk + 1) * 512], (), [hwn], hwn)
            for pair in range(2):
                for which in range(2):
                    ps, pn = P[it % 4], f"ps{it % 4}"
                    it += 1
                    col = which * 256 + pair * 128
                    for k in range(16):
                        S.mm(ps[:], Wq[:, k, col:col + 128], hw[:, k, :], k == 0, k == 15, ['Wq', hwn], [pn])
                    if which == 0:
                        S.act(QT[:, pair, blk * 512:(blk + 1) * 512], ps[:], AF.Identity, [pn], ['QT'], scale=0.125)
                    else:
                        S.copy('dve', KT[:, pair, blk * 512:(blk + 1) * 512], ps[:], [pn], ['KT'])
            for tt in range(8):
                ps, pn = P[4 + (tt % 4)], f"ps{4 + (tt % 4)}"
                for k in range(16):
                    S.mm(ps[0:64, 0:256], hw[:, k, tt * 64:(tt + 1) * 64], Wq[:, k, 512:768], k == 0, k == 15, ['Wq', hwn], [pn])
                S.copy('act' if tt % 2 else 'dve', V64[:, blk * 8 + tt, :], ps[0:64, 0:256], [pn], ['V64'])
    S.barrier()
    wa = [sb(f"wa{i}", [128, 64], F32) for i in range(4)]
    for half in range(2):
        psl = slice(half * 64, (half + 1) * 64)
        specs = [([[1, 64]], 8, -1, ALU.is_ge),
                 ([[1, 64]], -48, 0, ALU.is_ge),
                 ([[-1, 64]], 7, 1, ALU.is_ge),
                 ([[-1, 64]], 15, 0, ALU.is_ge)]
        for i, (pat, base, cm, op) in enumerate(specs):
            S.op('pool', lambda e, i=i, pat=pat, base=base, cm=cm, op=op, psl=psl: e.affine_select(
                wa[i][psl, :], cx.ones32[psl, 0:64], pat, op, 0.0, base=base, channel_multiplier=cm), ['ones32'], [f'wa{i}'])
    S.tt('dve', wa[0][:], wa[0][:], wa[1][:], ALU.max, ['wa0', 'wa1'], ['wa0'])
    S.tt('dve', wa[2][:], wa[2][:], wa[3][:], ALU.max, ['wa2', 'wa3'], ['wa2'])
    S.tt('dve', wa[0][:], wa[0][:], wa[2][:], ALU.mult, ['wa0', 'wa2'], ['wa0'])
    S.ts('dve', wa[0][:], wa[0][:], NBIG, -NBIG, ALU.mult, ALU.add, ['wa0'], ['wa0'])
    biasv = sb("biasv", [128, 2, 8, 512], F32)
    with contextlib.ExitStack() as es:
        braw = sb("braw", [128, 2, 15, 64], F32, es)
        for pair in range(2):
            S.dma(braw[:, pair], biasd[pair].rearrange("d p k -> p d k"), (), ['braw'], f'braw{pair}')
        for pair in range(2):
            for v in range(8):
                S.tt('dve', biasv[:, pair, v, :].rearrange("p (a k) -> p a k", k=64), braw[:, pair, v:v + 8, :],
                     wa[0][:].unsqueeze(1).broadcast_to([128, 8, 64]), ALU.add, ['braw', 'wa0'], ['biasv'])
        S.barrier()
    NCH = 4
    sc_ = [sb(f"sc{i}", [128, 512], F32) for i in range(NCH)]
    p32 = [sb(f"p32{i}", [128, 512], F32) for i in range(NCH)]
    pnb = [sb(f"pnb{i}", [128, 512], BF16) for i in range(NCH)]
    PT = [sb(f"PT{i}", [64, 8, 128], BF16) for i in range(NCH)]
    mx = [sb(f"mx{i}", [128, 1], F32) for i in range(NCH)]
    ssum = [sb(f"ssum{i}", [128, 1], F32) for i in range(NCH)]
    ob = [sb(f"ob{i}", [128, 2, 512], BF16) for i in range(2)]
    for b in range(2):
        tb = b * 4096
        for rr in range(0, 64, 2):
            chains = []
            for r in (rr, rr + 1):
                for pair in range(2):
                    chains.append((r, pair))
            info = []
            for ci, (r, pair) in enumerate(chains):
                r0 = min(max(r - 4, 0), 56)
                info.append(dict(r=r, pair=pair, v=r0 - r + 7, tq=tb + r * 64, tk=tb + r0 * 64, i=ci))
            obb, obn = ob[(rr // 8) % 2], f"ob{(rr // 8) % 2}"
            for c_ in info:
                i, pair = c_['i'], c_['pair']
                psS, psn = P[i], f"ps{i}"
                for hh in range(2):
                    hs = slice(hh * 64, (hh + 1) * 64)
                    S.mm(psS[hs, :], QT[hs, pair, c_['tq']:c_['tq'] + 64], KT[hs, pair, c_['tk']:c_['tk'] + 512], True, True,
                         ['QT', 'KT'], [psn])
            for c_ in info:
                i = c_['i']
                S.tt('dve', sc_[i][:], P[i][:], biasv[:, c_['pair'], c_['v'], :], ALU.add, [f"ps{i}", 'biasv'], [f"sc{i}"])
            for c_ in info:
                i = c_['i']
                S.op('dve', lambda e, i=i: e.tensor_reduce(mx[i][:], sc_[i][:], AX.X, ALU.max), [f"sc{i}"], [f"mx{i}"])
            for c_ in info:
                i = c_['i']
                S.ts('dve', mx[i][:], mx[i][:], -1.0, None, ALU.mult, None, [f"mx{i}"], [f"mx{i}"])
            for c_ in info:
                i = c_['i']
                S.act(p32[i][:], sc_[i][:], AF.Exp, [f"sc{i}", f"mx{i}"], [f"p32{i}", f"ssum{i}"], bias=mx[i][:], accum_out=ssum[i][:])
            for c_ in info:
                i = c_['i']
                S.op('dve', lambda e, i=i: e.reciprocal(ssum[i][:], ssum[i][:]), [f"ssum{i}"], [f"ssum{i}"])
            for c_ in info:
                i = c_['i']
                S.ts('dve', pnb[i][:], p32[i][:], ssum[i][:], None, ALU.mult, None, [f"p32{i}", f"ssum{i}"], [f"pnb{i}"])
            for c_ in info:
                i = c_['i']
                psT, ptn = P[4 + i % 2], f"ps{4 + i % 2}"
                ptb = psT[:].bitcast(BF16)
                for c in range(8):
                    S.transpose(ptb[0:64, c * 128:(c + 1) * 128], pnb[i][:, c * 64:(c + 1) * 64], cx.ident16[:],
                                [f"pnb{i}", 'ident16'], [ptn])
                S.copy('act', PT[i][:].rearrange("p c q -> p (c q)"), ptb[0:64, :], [ptn], [f"PT{i}"])
            for c_ in info:
                i, pair, r = c_['i'], c_['pair'], c_['r']
                psO, pon = P[6 + i % 2], f"ps{6 + i % 2}"
                for hh in range(2):
                    hs = slice(hh * 64, (hh + 1) * 64)
                    hc = (pair * 2 + hh) * 64
                    for c in range(8):
                        S.mm(psO[hs, 0:64], V64[:, c_['tk'] // 64 + c, hc:hc + 64], PT[i][:, c, hs], c == 0, c == 7,
                             ['V64', f"PT{i}"], [pon])
                S.copy('act', obb[:, pair, (r % 8) * 64:(r % 8 + 1) * 64], psO[:, 0:64], [pon], [obn])
            if (rr + 1) % 8 == 7:
                S.dma(oTv[:, :, tb + (rr // 8) * 512: tb + (rr // 8 + 1) * 512], obb[:], [obn], ['oT'], f'o{obn}')
    return cx.finish(['oT'])


_PROGS = {}


def _prog(key, fn):
    if key not in _PROGS:
        _PROGS[key] = fn()
    return _PROGS[key]


def _run(nc, in_maps):
    res = run_bass_kernel_spmd(nc, in_maps, core_ids=list(range(8)))
    return res.results


def kernel(x, c, ada_w, ada_b, norm_mix, norm_ffn, ssd_w_in, ssd_conv_w, ssd_conv_b, ssd_a_log,
           ssd_dt_bias, ssd_d, ssd_norm_w, ssd_w_out, na_w_qkv, na_rpb, na_w_o,
           moe_w_group, moe_w_expert, moe_w1, moe_w3, moe_w2, final_norm):
    f32 = np.float32
    A = lambda a: np.ascontiguousarray(np.asarray(a, dtype=f32))
    x = A(x); c = A(c); ada_w = A(ada_w); ada_b = A(ada_b)
    xf = x.reshape(8192, 2048)
    cT = np.ascontiguousarray(c.T.reshape(16, 128, 2).transpose(1, 0, 2))
    nc = _prog('ada', build_ada)
    r = _run(nc, [dict(cT=cT, aw=np.ascontiguousarray(ada_w[:, :, j * 1536:(j + 1) * 1536]),
                       ab=np.ascontiguousarray(ada_b[:, j * 1536:(j + 1) * 1536])) for j in range(8)])
    mod = np.concatenate([np.asarray(q['mod']) for q in r], axis=2)
    nm = A(norm_mix); nf = A(norm_ffn); fn_ = A(final_norm)
    nc = _prog('t0', build_t0)
    maps = []
    for i in range(8):
        b = i // 4
        rows = np.stack([mod[0, b, 0:2048], mod[0, b, 2048:4096], nm[0]])
        maps.append(dict(x=np.ascontiguousarray(xf[i * 1024:(i + 1) * 1024]), rows=np.ascontiguousarray(rows)))
    r = _run(nc, maps)
    hT = np.ascontiguousarray(np.concatenate([np.asarray(q['hT']) for q in r], axis=1))
    w_in = A(ssd_w_in)[0]; cw = A(ssd_conv_w)[0]; cbv = A(ssd_conv_b)[0]
    alog = A(ssd_a_log)[0]; dtb = A(ssd_dt_bias)[0]; dsk = A(ssd_d)[0]; snw = A(ssd_norm_w)[0]
    nc = _prog('ssd', build_ssd)
    maps = []
    for g in range(8):
        cols = np.concatenate([np.arange(g * 512, (g + 1) * 512), 4096 + np.arange(g * 512, (g + 1) * 512),
                               8192 + np.arange(g * 128, (g + 1) * 128), 9216 + np.arange(g * 128, (g + 1) * 128),
                               10240 + np.arange(g * 8, (g + 1) * 8), 10304 + np.arange(g * 8, (g + 1) * 8)])
        ch = np.concatenate([np.arange(g * 512, (g + 1) * 512), 4096 + np.arange(g * 128, (g + 1) * 128),
                             5120 + np.arange(g * 128, (g + 1) * 128)])
        hs = slice(g * 8, (g + 1) * 8)
        hp = np.zeros((3, 16), f32)
        hp[0, 0:8] = dtb[0, hs]; hp[0, 8:16] = dtb[1, hs]
        hp[1, 0:8] = alog[0, hs]; hp[1, 8:16] = alog[1, hs]
        hp[2, 0:8] = dsk[hs]
        maps.append(dict(hT=hT, Wg=np.ascontiguousarray(w_in[:, cols]), cw=np.ascontiguousarray(cw[:, ch].T),
                         cb=np.ascontiguousarray(cbv[ch][:, None]), hp=hp,
                         nw=np.ascontiguousarray(snw[g * 512:(g + 1) * 512][None, :])))
    r = _run(nc, maps)
    ynT = np.concatenate([np.asarray(q['ynT']) for q in r], axis=0)
    def c_maps(l, xin, mixT, wout, last):
        wr = np.ascontiguousarray(np.concatenate([A(moe_w_group)[l], A(moe_w_expert)[l]], axis=1))
        w1 = A(moe_w1)[l]; w3 = A(moe_w3)[l]; w2 = A(moe_w2)[l]
        maps = []
        for i in range(8):
            b = i // 4
            m = mod[l, b]
            if last:
                nxt = [fn_, fn_, fn_]
            else:
                nxt = [mod[l + 1, b, 0:2048], mod[l + 1, b, 2048:4096], nm[l + 1]]
            rows = np.stack([m[4096:6144], m[6144:8192], m[8192:10240], m[10240:12288], nf[l]] + nxt)
            maps.append(dict(x=np.ascontiguousarray(xin[i * 1024:(i + 1) * 1024]),
                             mixT=np.ascontiguousarray(mixT[:, i * 1024:(i + 1) * 1024]),
                             wout=wout, rows=np.ascontiguousarray(rows), wr=wr, w1=w1, w3=w3, w2=w2))
        return maps
    nc = _prog('c0', lambda: build_c(4096, False))
    r = _run(nc, c_maps(0, xf, ynT, A(ssd_w_out)[0], False))
    x1 = np.concatenate([np.asarray(q['xo']) for q in r], axis=0)
    hT1 = np.ascontiguousarray(np.concatenate([np.asarray(q['hTn']) for q in r], axis=1))
    wqkv = A(na_w_qkv)[0]; rpb = A(na_rpb)[0]
    jq = np.arange(64)[:, None]; kc = np.arange(64)[None, :]
    dx = kc - jq + 15
    ok = (dx >= 0) & (dx <= 30)
    dxc = np.clip(dx, 0, 30)
    nc = _prog('na', build_na)
    maps = []
    for j in range(8):
        Wq = np.concatenate([wqkv[:, w * 2048 + j * 256: w * 2048 + (j + 1) * 256] for w in range(3)], axis=1)
        bias = np.zeros((2, 15, 128, 64), f32)
        for pair in range(2):
            for hh in range(2):
                h = 4 * j + pair * 2 + hh
                g_ = rpb[h][:, dxc]
                g_ = np.where(ok[None], g_, f32(0))
                bias[pair, :, hh * 64:(hh + 1) * 64, :] = g_
        maps.append(dict(hT=hT1, Wq=np.ascontiguousarray(Wq), bias=bias))
    r = _run(nc, maps)
    oT = np.concatenate([np.asarray(q['oT']) for q in r], axis=0)
    nc = _prog('c1', lambda: build_c(2048, True))
    r = _run(nc, c_maps(1, x1, oT, A(na_w_o)[0], True))
    out = np.concatenate([np.asarray(q['out']) for q in r], axis=0)
    return out.reshape(2, 4096, 2048).astype(f32)
```

```python
import contextlib
import numpy as np
import concourse.bass as bass
import concourse.mybir as mybir
from concourse.bass_utils import run_bass_kernel_spmd

F32 = mybir.dt.float32
BF16 = mybir.dt.bfloat16
I32 = mybir.dt.int32
ALU = mybir.AluOpType
AF = mybir.ActivationFunctionType
AX = mybir.AxisListType


class Sched:
    ENGS = ['pe', 'act', 'dve', 'pool', 'sp']

    def __init__(self, nc, ctx):
        self.nc = nc
        self.ctx = ctx
        self.q = {e: [] for e in self.ENGS}
        self.cnt = {e: 0 for e in self.ENGS}
        self.sems = {e: ctx.enter_context(nc.semaphore('s_' + e)) for e in self.ENGS}
        self.dsem = {}
        self.lastw = {}
        self.readers = {}
        self.seen = {e: {} for e in self.ENGS}
        self.nwaits = 0

    def _semh(self, key):
        if key[0] == 'e':
            return self.sems[key[1]]
        return self.dsem[key[1]][0]

    def op(self, eng, fn, reads=(), writes=(), dma=None, pe_acc=False, inc=16):
        deps = {}
        def add(sig):
            k, v, e = sig[:3]
            if pe_acc and eng == 'pe' and e == 'pe' and k[0] == 'e':
                return
            if deps.get(k, 0) < v:
                deps[k] = v
        for r in reads:
            if r in self.lastw:
                add(self.lastw[r])
        for w in writes:
            if w in self.lastw:
                add(self.lastw[w])
            for rd in self.readers.get(w, ()):
                add(rd)
        waits = []
        for k, v in deps.items():
            if self.seen[eng].get(k, 0) >= v:
                continue
            self.seen[eng][k] = v
            waits.append((k, v))
        if dma is not None:
            if dma not in self.dsem:
                self.dsem[dma] = [self.ctx.enter_context(self.nc.semaphore('d_' + dma)), 0]
            self.dsem[dma][1] += inc
            sig = (('d', dma), self.dsem[dma][1], eng, inc)
        else:
            self.cnt[eng] += 1
            sig = (('e', eng), self.cnt[eng], eng)
        for w in writes:
            self.lastw[w] = sig
            self.readers[w] = []
        for r in reads:
            if r not in writes:
                self.readers.setdefault(r, []).append(sig)
        self.q[eng].append((fn, waits, sig))
        self.nwaits += len(waits)

    def barrier(self):
        sigs = []
        for e in self.ENGS:
            if self.cnt[e] > 0:
                sigs.append((('e', e), self.cnt[e]))
        for name, (h, c) in self.dsem.items():
            if c > 0:
                sigs.append((('d', name), c))
        for e in self.ENGS:
            waits = []
            for k, v in sigs:
                if self.seen[e].get(k, 0) >= v:
                    continue
                self.seen[e][k] = v
                waits.append((k, v))
            if waits:
                self.q[e].append((None, waits, None))
        self.lastw = {}
        self.readers = {}

    def emit(self):
        nc = self.nc
        with nc.Block() as block:
            def mk(ename):
                def body(eng):
                    for fn, waits, sig in self.q[ename]:
                        for k, v in waits:
                            eng.wait_ge(self._semh(k), v)
                        if fn is None:
                            continue
                        ins = fn(eng)
                        k = sig[0]
                        if k[0] == 'e':
                            ins.then_inc(self.sems[ename], 1)
                        else:
                            ins.then_inc(self.dsem[k[1]][0], sig[3])
                return body
            block.tensor(mk('pe'))
            block.scalar(mk('act'))
            block.vector(mk('dve'))
            block.gpsimd(mk('pool'))
            block.sync(mk('sp'))

    def dma(self, out, in_, reads, writes, sem, eng='sp', **kw):
        self.op(eng, lambda e: e.dma_start(out=out, in_=in_, **kw), reads, writes, dma=sem)

    def cc(self, kind, groups, in_ap, out_ap, reads, writes, sem):
        self.op('pool', lambda e: e.collective_compute(kind, ALU.bypass, replica_groups=groups, ins=[in_ap], outs=[out_ap]),
                reads, writes, dma=sem, inc=1)

    def mm(self, out, lhsT, rhs, start, stop, reads, writes):
        self.op('pe', lambda e: e.matmul(out, lhsT, rhs, start=start, stop=stop), reads, writes,
                pe_acc=not start)

    def transpose(self, out, in_, ident, reads, writes):
        self.op('pe', lambda e: e.transpose(out, in_, ident), reads, writes)

    def act(self, out, in_, func, reads, writes, bias=None, scale=None, accum_out=None, eng='act'):
        kw = {}
        if bias is not None:
            kw['bias'] = bias
        if scale is not None:
            kw['scale'] = scale
        if accum_out is not None:
            kw['accum_out'] = accum_out
        self.op(eng, lambda e: e.activation(out, in_, func, **kw), reads, writes)

    def tt(self, eng, out, in0, in1, op, reads, writes):
        self.op(eng, lambda e: e.tensor_tensor(out, in0, in1, op), reads, writes)

    def ts(self, eng, out, in0, s1, s2, op0, op1, reads, writes, accum_out=None):
        if op1 is None:
            self.op(eng, lambda e: e.tensor_scalar(out, in0, s1, None, op0), reads, writes)
        elif accum_out is not None:
            self.op(eng, lambda e: e.tensor_scalar(out, in0, s1, s2, op0, op1, accum_out=accum_out), reads, writes)
        else:
            self.op(eng, lambda e: e.tensor_scalar(out, in0, s1, s2, op0, op1), reads, writes)

    def stt(self, eng, out, in0, scalar, in1, op0, op1, reads, writes):
        self.op(eng, lambda e: e.scalar_tensor_tensor(out, in0, scalar, in1, op0, op1), reads, writes)

    def copy(self, eng, out, in_, reads, writes):
        if eng == 'act':
            self.op(eng, lambda e: e.copy(out, in_), reads, writes)
        else:
            self.op(eng, lambda e: e.tensor_copy(out, in_), reads, writes)

    def memset(self, eng, ap, val, writes):
        self.op(eng, lambda e: e.memset(ap, val), (), writes)


D = 2048
EPS = 1e-6


class Ctx:
    def __init__(self):
        self.nc = bass.Bass("TRN2", target_bir_lowering=False)
        self.es = contextlib.ExitStack()
        self.S = Sched(self.nc, self.es)
        self.ps = [self.es.enter_context(self.nc.psum_tensor(f"ps{i}", [128, 512], F32)) for i in range(8)]
        self.uid = 0

    def din(self, name, shape, dt):
        return self.nc.dram_tensor(name, list(shape), dt, kind="ExternalInput").ap()

    def dout(self, name, shape, dt):
        return self.nc.dram_tensor(name, list(shape), dt, kind="ExternalOutput").ap()

    def sb(self, name, shape, dt, es=None):
        self.uid += 1
        return (es or self.es).enter_context(self.nc.sbuf_tensor(f"{name}_u{self.uid}", list(shape), dt))

    def consts(self):
        S = self.S
        self.ones32 = self.sb("ones32", [128, 128], F32)
        self.ident32 = self.sb("ident32", [128, 128], F32)
        self.ident16 = self.sb("ident16", [128, 128], BF16)
        S.memset('pool', self.ones32[:], 1.0, ['ones32'])
        S.op('pool', lambda e: e.affine_select(self.ident32[:], self.ones32[:], [[-1, 128]], ALU.is_equal, 0.0,
                                                base=0, channel_multiplier=1), ['ones32'], ['ident32'])
        S.op('pool', lambda e: e.affine_select(self.ident16[:], self.ones32[:], [[-1, 128]], ALU.is_equal, 0.0,
                                                base=0, channel_multiplier=1), ['ones32'], ['ident16'])

    def mask(self, name, cmp, base, dt=F32, pmul=1, fmul=-1, es=None):
        m = self.sb(name, [128, 128], dt, es)
        self.S.op('pool', lambda e: e.affine_select(m[:], self.ones32[:], [[fmul, 128]], cmp, 0.0, base=base,
                                                    channel_multiplier=pmul), ['ones32'], [name])
        return m

    def finish(self, out_res):
        S = self.S
        S.barrier()
        S.op('sp', lambda e: e.nop(), [], [])
        S.emit()
        self.es.close()
        return self.nc


def norm_tile(cx, x_ap, xres, wmod, shb, out_ap, outres, tag, tmp32=None):
    S = cx.S
    junk, ssq, rstd = cx.nt_junk, cx.nt_ssq, cx.nt_rstd
    S.act(junk[:], x_ap, AF.Square, xres, ['nt_junk', 'nt_ssq'], accum_out=ssq[:])
    S.ts('dve', rstd[:], ssq[:], 1.0 / D, EPS, ALU.mult, ALU.add, ['nt_ssq'], ['nt_rstd'])
    S.act(rstd[:], rstd[:], AF.Sqrt, ['nt_rstd'], ['nt_rstd'])
    S.op('dve', lambda e: e.reciprocal(rstd[:], rstd[:]), ['nt_rstd'], ['nt_rstd'])
    if shb is None:
        S.stt('dve', out_ap, x_ap, rstd[:], wmod[0][:], ALU.mult, ALU.mult, xres + ['nt_rstd', wmod[1]], outres)
    else:
        t = tmp32 if tmp32 is not None else junk
        tr = ['nt_junk'] if tmp32 is None else [tag + '_tmp']
        S.stt('dve', t[:], x_ap, rstd[:], wmod[0][:], ALU.mult, ALU.mult, xres + ['nt_rstd', wmod[1]], tr)
        S.tt('pool', out_ap, t[:], shb[0][:], ALU.add, tr + [shb[1]], outres)


def norm_alloc(cx):
    cx.nt_junk = cx.sb("nt_junk", [128, 2048], F32)
    cx.nt_ssq = cx.sb("nt_ssq", [128, 1], F32)
    cx.nt_rstd = cx.sb("nt_rstd", [128, 1], F32)


def load_row(cx, name, src_row_ap, es=None):
    t = cx.sb(name, [128, 2048], F32, es)
    cx.S.dma(t[:], src_row_ap.partition_broadcast(128), (), [name], 'row_' + name)
    return t


def make_wmod(cx, name, scb_name, sc_row, nw_row, es=None):
    scb = load_row(cx, scb_name, sc_row, es)
    nwb = load_row(cx, name, nw_row, es)
    cx.S.stt('dve', nwb[:], scb[:], 1.0, nwb[:], ALU.add, ALU.mult, [scb_name, name], [name])
    return nwb


def build_ada():
    cx = Ctx()
    S = cx.S
    cT = cx.din("cT", [128, 16, 2], F32)
    aw = cx.din("aw", [2, 2048, 1536], F32)
    ab = cx.din("ab", [2, 1536], F32)
    mod = cx.dout("mod", [2, 2, 1536], F32)
    cs = cx.sb("cs", [128, 16, 2], F32)
    S.dma(cs[:], cT, (), ['cs'], 'cs')
    S.act(cs[:], cs[:], AF.Silu, ['cs'], ['cs'])
    wsl = [cx.sb(f"aw{i}", [128, 16, 512], F32) for i in range(2)]
    bsb = cx.sb("bsb", [2, 2, 1536], F32)
    for l in range(2):
        S.dma(bsb[:, l, :], ab[l, :].partition_broadcast(2), (), [f'bsb{l}'], f'bsb{l}')
    osb = cx.sb("osb", [2, 2, 1536], F32)
    it = 0
    for l in range(2):
        for blk in range(3):
            w = wsl[it % 2]
            wn = f"aw{it % 2}"
            S.dma(w[:], aw[l, :, blk * 512:(blk + 1) * 512].rearrange("(c p) n -> p c n", p=128), (), [wn], wn)
            ps = cx.ps[it % 2]
            pn = f"ps{it % 2}"
            for k in range(16):
                S.mm(ps[0:2, :], cs[:, k, :], w[:, k, :], k == 0, k == 15, ['cs', wn], [pn])
            S.tt('dve', osb[:, l, blk * 512:(blk + 1) * 512], ps[0:2, :], bsb[:, l, blk * 512:(blk + 1) * 512], ALU.add,
                 [pn, f'bsb{l}'], ['osb'])
            it += 1
    S.dma(mod.rearrange("l b n -> b l n"), osb[:], ['osb'], ['mod'], 'out')
    return cx.finish(['mod'])


def emit_transposes_bf16(cx, src_tile, src_res, dst, dst_res, j, bank0=0, perm=False):
    S = cx.S
    for half in range(2):
        ps = cx.ps[bank0 + half]
        pn = f"ps{bank0 + half}"
        pb = ps[:].bitcast(BF16)
        for cc in range(8):
            c = half * 8 + cc
            srcv = src_tile[:, c:2048:16] if perm else src_tile[:, c * 128:(c + 1) * 128]
            S.transpose(pb[:, cc * 128:(cc + 1) * 128], srcv, cx.ident16[:],
                        src_res + ['ident16'], [pn])
        eng = 'act' if half == 0 else 'dve'
        S.copy(eng, dst[:, half * 8:(half + 1) * 8, j * 128:(j + 1) * 128],
               pb.rearrange("p (c t) -> p c t", t=128), [pn], dst_res)


def build_t0():
    cx = Ctx()
    S = cx.S
    x = cx.din("x", [1024, 2048], F32)
    rows = cx.din("rows", [3, 2048], F32)
    hT = cx.dout("hT", [2048, 1024], BF16)
    cx.consts()
    norm_alloc(cx)
    wmod = make_wmod(cx, "wmod", "scb", rows[1, :], rows[2, :])
    shb = load_row(cx, "shb", rows[0, :])
    xs = [cx.sb(f"xt{i}", [128, 2048], F32) for i in range(2)]
    hb = [cx.sb(f"hb{i}", [128, 2048], BF16) for i in range(2)]
    hTb = cx.sb("hTb", [128, 16, 1024], BF16)
    for j in range(8):
        xt, xn = xs[j % 2], f"xt{j % 2}"
        S.dma(xt[:], x[j * 128:(j + 1) * 128, :], (), [xn], xn)
        norm_tile(cx, xt[:], [xn], (wmod, 'wmod'), (shb, 'shb'), hb[j % 2][:], [f"hb{j % 2}"], 't0')
        emit_transposes_bf16(cx, hb[j % 2], [f"hb{j % 2}"], hTb, ['hTb'], j, bank0=(j % 2) * 2)
    S.dma(hT.rearrange("(c p) t -> p c t", p=128), hTb[:], ['hTb'], ['hT'], 'out')
    return cx.finish(['hT'])


NE = 32
DEBUG_C = False
CAP = 128
BIG = 30000.0


def build_c(K, last):
    KC = K // 128
    cx = Ctx()
    S = cx.S
    nc = cx.nc
    x_in = cx.din("x", [1024, 2048], F32)
    mixT = cx.din("mixT", [K, 1024], BF16)
    wout = cx.din("wout", [K, 2048], F32)
    rows = cx.din("rows", [8, 2048], F32)
    wr = cx.din("wr", [2048, 36], F32)
    w1 = cx.din("w1", [NE, 2048, 512], F32)
    w3 = cx.din("w3", [NE, 2048, 512], F32)
    w2 = cx.din("w2", [NE, 512, 2048], F32)
    if last:
        xo = cx.dout("out", [1024, 2048], F32)
    else:
        xo = cx.dout("xo", [1024, 2048], F32)
        hTn = cx.dout("hTn", [2048, 1024], BF16)
    cx.consts()
    xs = cx.sb("xs", [128, 8, 2048], F32)
    S.dma(xs[:], x_in.rearrange("(j p) d -> p j d", p=128), (), ['xs'], 'xs')
    XR = [f'xs{j}' for j in range(8)]
    for j in range(8):
        S.lastw[XR[j]] = S.lastw['xs']

    with contextlib.ExitStack() as es:
        mT = cx.sb("mT", [128, KC, 1024], BF16, es)
        S.dma(mT[:], mixT.rearrange("(c p) t -> p c t", p=128), (), ['mT'], 'mT')
        g1b = load_row(cx, "g1b", rows[0, :], es)
        wsl = [cx.sb(f"wo{i}", [128, KC, 512], BF16, es) for i in range(2)]
        tmp = [cx.sb(f"optmp{i}", [128, 512], F32, es) for i in range(2)]
        it = 0
        for n in range(4):
            w, wn = wsl[n % 2], f"wo{n % 2}"
            S.dma(w[:], wout[:, n * 512:(n + 1) * 512].rearrange("(c p) n -> p c n", p=128), (), [wn], wn, eng='pool')
            for j in range(8):
                ps, pn = cx.ps[it % 4], f"ps{it % 4}"
                for k in range(KC):
                    S.mm(ps[:], mT[:, k, j * 128:(j + 1) * 128], w[:, k, :], k == 0, k == KC - 1, ['mT', wn], [pn])
                t, tn = tmp[it % 2], f"optmp{it % 2}"
                S.tt('dve', t[:], ps[:], g1b[:, n * 512:(n + 1) * 512], ALU.mult, [pn, 'g1b'], [tn])
                S.tt('pool', xs[:, j, n * 512:(n + 1) * 512], xs[:, j, n * 512:(n + 1) * 512], t[:], ALU.add,
                     [tn, XR[j]], [XR[j]])
                it += 1
    S.barrier()

    hTb = cx.sb("hTbm", [128, 16, 1024], BF16)
    lg = cx.sb("lg", [128, 8, 36], F32)
    with contextlib.ExitStack() as es:
        norm_alloc_es(cx, es)
        wmod2 = make_wmod(cx, "wmod2", "sc2b", rows[2, :], rows[4, :], es)
        sh2b = load_row(cx, "sh2b", rows[1, :], es)
        wrs = cx.sb("wrs", [128, 16, 36], F32, es)
        S.dma(wrs[:], wr.rearrange("(c p) n -> p c n", p=128), (), ['wrs'], 'wrs')
        h32 = [cx.sb(f"h32_{i}", [128, 2048], F32, es) for i in range(2)]
        hbt = [cx.sb(f"hbt_{i}", [128, 2048], BF16, es) for i in range(2)]
        hT32 = [cx.sb(f"hT32_{i}", [128, 16, 128], F32, es) for i in range(2)]
        for j in range(8):
            h, hn = h32[j % 2], f"h32_{j % 2}"
            norm_tile(cx, xs[:, j, :], [XR[j]], (wmod2, 'wmod2'), (sh2b, 'sh2b'), h[:], [hn], 'n2')
            S.copy('act', hbt[j % 2][:], h[:], [hn], [f'hbt{j % 2}'])
            emit_transposes_bf16(cx, hbt[j % 2], [f'hbt{j % 2}'], hTb, ['hTbm'], j, bank0=6, perm=True)
            hT, hTn_ = hT32[j % 2], f"hT32_{j % 2}"
            for q in range(4):
                ps, pn = cx.ps[q], f"ps{q}"
                for cc in range(4):
                    c = q * 4 + cc
                    S.transpose(ps[:, cc * 128:(cc + 1) * 128], h[:, c * 128:(c + 1) * 128], cx.ident32[:],
                                [hn, 'ident32'], [pn])
                S.copy('dve' if q % 2 else 'act', hT[:, q * 4:(q + 1) * 4, :], ps[:].rearrange("p (c t) -> p c t", t=128),
                       [pn], [hTn_])
            pl, pln = cx.ps[4 + (j % 2)], f"ps{4 + (j % 2)}"
            for c in range(16):
                S.mm(pl[:, 0:36], hT[:, c, :], wrs[:, c, :], c == 0, c == 15, [hTn_, 'wrs'], [pln])
            S.copy('dve', lg[:, j, :], pl[:, 0:36], [pln], ['lg'])
    S.barrier()

    Wt = cx.sb("Wt", [128, 8, 32], F32)
    with contextlib.ExitStack() as es:
        def t(name, shape, dt=F32):
            return cx.sb(name, shape, dt, es)
        R = ['rt']
        gl = lg[:, :, 0:4]
        el = lg[:, :, 4:36]
        gmax = t("gmax", [128, 8]); gsh = t("gsh", [128, 8, 4]); gsum = t("gsum", [128, 8]); gw = t("gw", [128, 8])
        goh = t("goh", [128, 8, 4]); pen = t("pen", [128, 8, 4]); elm = t("elm", [128, 8, 32])
        m1 = t("m1", [128, 8]); is1 = t("is1", [128, 8, 32]); m2 = t("m2", [128, 8]); is2 = t("is2", [128, 8, 32])
        dd = t("dd", [128, 8]); p1 = t("p1", [128, 8]); p2 = t("p2", [128, 8]); tmpa = t("tmpa", [128, 8, 32])
        A = t("A", [128, 8, 32]); A16 = t("A16", [128, 8, 32], BF16)
        def V(fn):
            S.op('dve', fn, ['lg'] + R, R)
        def bc(ap, n):
            return ap.unsqueeze(2).broadcast_to([128, 8, n])
        V(lambda e: e.tensor_reduce(gmax[:], gl, AX.X, ALU.max))
        V(lambda e: e.tensor_tensor(gsh[:], gl, bc(gmax[:], 4), ALU.subtract))
        V(lambda e: e.tensor_tensor(goh[:], gl, bc(gmax[:], 4), ALU.is_equal))
        S.act(gsh[:], gsh[:], AF.Exp, R, R)
        V(lambda e: e.tensor_reduce(gsum[:], gsh[:], AX.X, ALU.add))
        V(lambda e: e.reciprocal(gw[:], gsum[:]))
        V(lambda e: e.tensor_scalar(pen[:], goh[:], BIG, -BIG, ALU.mult, ALU.add))
        V(lambda e: e.tensor_tensor(elm[:].rearrange("p j (g k) -> p j g k", k=8), el.rearrange("p j (g k) -> p j g k", k=8),
                                    pen[:].unsqueeze(3).broadcast_to([128, 8, 4, 8]), ALU.add))
        V(lambda e: e.tensor_reduce(m1[:], elm[:], AX.X, ALU.max))
        V(lambda e: e.tensor_tensor(is1[:], elm[:], bc(m1[:], 32), ALU.is_equal))
        V(lambda e: e.scalar_tensor_tensor(elm[:], is1[:], -BIG, elm[:], ALU.mult, ALU.add))
        V(lambda e: e.tensor_reduce(m2[:], elm[:], AX.X, ALU.max))
        V(lambda e: e.tensor_tensor(is2[:], elm[:], bc(m2[:], 32), ALU.is_equal))
        V(lambda e: e.tensor_tensor(dd[:], m2[:], m1[:], ALU.subtract))
        S.act(dd[:], dd[:], AF.Exp, R, R)
        V(lambda e: e.tensor_scalar(p1[:], dd[:], 1.0, None, ALU.add))
        V(lambda e: e.reciprocal(p1[:], p1[:]))
        V(lambda e: e.tensor_tensor(p2[:], dd[:], p1[:], ALU.mult))
        V(lambda e: e.tensor_tensor(p1[:], p1[:], gw[:], ALU.mult))
        V(lambda e: e.tensor_tensor(p2[:], p2[:], gw[:], ALU.mult))
        V(lambda e: e.tensor_tensor(Wt[:], is1[:], bc(p1[:], 32), ALU.mult))
        V(lambda e: e.tensor_tensor(tmpa[:], is2[:], bc(p2[:], 32), ALU.mult))
        V(lambda e: e.tensor_tensor(Wt[:], Wt[:], tmpa[:], ALU.add))
        V(lambda e: e.tensor_tensor(A[:], is1[:], is2[:], ALU.add))
        V(lambda e: e.tensor_copy(A16[:], A[:]))
    if DEBUG_C:
        dlg = cx.dout("dlg", [128, 8, 36], F32)
        dWt = cx.dout("dWt", [128, 8, 32], F32)
        S.dma(dlg, lg[:], ['lg'], ['dlg'], 'dlg')
        S.dma(dWt, Wt[:], ['rt'], ['dWt'], 'dWt')
    S.barrier()

    xscr = nc.dram_tensor("xscr", [1024, 2048], F32).ap()
    S.dma(xscr.rearrange("(j p) d -> p j d", p=128), xs[:], XR, ['xscr'], 'xscr')
    for j in range(8):
        S.memset('pool' if j % 2 else 'dve', xs[:, j, :], 0.0, [XR[j]])

    with contextlib.ExitStack() as es:
        NW = 5
        wslot = [cx.sb(f"wsl{i}", [128, 8192], BF16, es) for i in range(NW)]
        s1 = [cx.sb(f"s1_{i}", [128, 512], F32, es) for i in range(2)]
        actT = cx.sb("actT", [128, 4, 1024], BF16, es)
        ytmp = [cx.sb(f"ytmp{i}", [128, 512], F32, es) for i in range(4)]
        widx = [0]

        def wload(kind, e):
            src = {'w1': w1, 'w3': w3, 'w2': w2}[kind]
            i = widx[0] % NW
            widx[0] += 1
            wn = f"wsl{i}"
            nn = 2048 if kind == 'w2' else 512
            dst = wslot[i][:].rearrange("p (c n) -> p c n", n=nn)
            if kind == 'w2':
                S.dma(dst, src[e].rearrange("(c p) n -> p c n", p=128), (), [wn], wn, eng='pool')
            else:
                S.dma(dst, src[e].rearrange("(p c) n -> p c n", c=16), (), [wn], wn, eng='pool')
            return (wslot[i], wn)

        cur = [wload('w1', 0), wload('w3', 0), wload('w2', 0)]
        it = 0
        for e_ in range(NE):
            (w1s, w1n), (w3s, w3n), (w2s, w2n) = cur
            nxt = [None, None, None]
            if e_ + 1 < NE:
                nxt[0] = wload('w1', e_ + 1)
                nxt[1] = wload('w3', e_ + 1)
            w1v = w1s[:].rearrange("p (c n) -> p c n", n=512)
            w3v = w3s[:].rearrange("p (c n) -> p c n", n=512)
            w2v = w2s[:].rearrange("p (c n) -> p c n", n=2048)
            for th in range(2):
                for fb in range(4):
                    pA, pan = cx.ps[(it % 2) * 2], f"ps{(it % 2) * 2}"
                    pB, pbn = cx.ps[(it % 2) * 2 + 1], f"ps{(it % 2) * 2 + 1}"
                    for k in range(16):
                        S.mm(pA[:], w1v[:, k, fb * 128:(fb + 1) * 128], hTb[:, k, th * 512:(th + 1) * 512], k == 0, k == 15,
                             [w1n, 'hTbm'], [pan])
                    for k in range(16):
                        S.mm(pB[:], w3v[:, k, fb * 128:(fb + 1) * 128], hTb[:, k, th * 512:(th + 1) * 512], k == 0, k == 15,
                             [w3n, 'hTbm'], [pbn])
                    ss, sn = s1[it % 2], f"s1_{it % 2}"
                    S.act(ss[:], pA[:], AF.Silu, [pan], [sn])
                    S.tt('dve', actT[:, fb, th * 512:(th + 1) * 512], ss[:], pB[:], ALU.mult, [sn, pbn], [f'actT{th}'])
                    it += 1
            if e_ + 1 < NE:
                nxt[2] = wload('w2', e_ + 1)
            for j in range(8):
                for n in range(4):
                    pY, pyn = cx.ps[4 + (it % 4)], f"ps{4 + (it % 4)}"
                    for fb in range(4):
                        S.mm(pY[:], actT[:, fb, j * 128:(j + 1) * 128], w2v[:, fb, n * 512:(n + 1) * 512], fb == 0, fb == 3,
                             [f'actT{j // 4}', w2n], [pyn])
                    yt, ytn = ytmp[it % 4], f"ytmp{it % 4}"
                    S.act(yt[:], pY[:], AF.Identity, [pyn], [ytn], scale=Wt[:, j, e_:e_ + 1])
                    S.tt('pool' if it % 2 == 0 else 'dve', xs[:, j, n * 512:(n + 1) * 512], xs[:, j, n * 512:(n + 1) * 512],
                         yt[:], ALU.add, [ytn, XR[j]], [XR[j]])
                    it += 1
            cur = nxt
    S.barrier()

    with contextlib.ExitStack() as es:
        norm_alloc_es(cx, es)
        g2b = load_row(cx, "g2b", rows[3, :], es)
        xre = [cx.sb(f"xre{i}", [128, 2048], F32, es) for i in range(2)]
        for j in range(8):
            xt_, xtn = xre[j % 2], f"xre{j % 2}"
            S.dma(xt_[:], xscr[j * 128:(j + 1) * 128, :], ['xscr'], [xtn], xtn)
            S.tt('dve', xs[:, j, :], xs[:, j, :], g2b[:], ALU.mult, [XR[j], 'g2b'], [XR[j]])
            S.tt('pool', xs[:, j, :], xs[:, j, :], xt_[:], ALU.add, [XR[j], xtn], [XR[j]])
        if last:
            nwb = load_row(cx, "nwN", rows[7, :], es)
            ob = [cx.sb(f"ob{i}", [128, 2048], F32, es) for i in range(2)]
            for j in range(8):
                o, on = ob[j % 2], f"ob{j % 2}"
                norm_tile(cx, xs[:, j, :], [XR[j]], (nwb, 'nwN'), None, o[:], [on], 'nf')
                S.dma(xo[j * 128:(j + 1) * 128, :], o[:], [on], ['xo'], f'outd{j % 2}')
        else:
            S.dma(xo.rearrange("(j p) d -> p j d", p=128), xs[:], XR, ['xo'], 'outx')
            wmodN = make_wmod(cx, "wmodN", "scNb", rows[6, :], rows[7, :], es)
            shNb = load_row(cx, "shNb", rows[5, :], es)
            hbn = [cx.sb(f"hbn{i}", [128, 2048], BF16, es) for i in range(2)]
            hTb = cx.sb("hTb", [128, 16, 1024], BF16, es)
            for j in range(8):
                norm_tile(cx, xs[:, j, :], [XR[j]], (wmodN, 'wmodN'), (shNb, 'shNb'), hbn[j % 2][:], [f"hbn{j % 2}"], 'nn')
                emit_transposes_bf16(cx, hbn[j % 2], [f"hbn{j % 2}"], hTb, ['hTb'], j, bank0=(j % 2) * 2)
            S.dma(hTn.rearrange("(c p) t -> p c t", p=128), hTb[:], ['hTb'], ['hTn'], 'outh')
    return cx.finish(['xo'] + ([] if last else ['hTn']) + (['dlg', 'dWt'] if DEBUG_C else []))


def norm_alloc_es(cx, es):
    cx.nt_junk = cx.sb("nt_junk", [128, 2048], F32, es)
    cx.nt_ssq = cx.sb("nt_ssq", [128, 1], F32, es)
    cx.nt_rstd = cx.sb("nt_rstd", [128, 1], F32, es)


def build_ssd():
    cx = Ctx()
    S = cx.S
    hT = cx.din("hT", [2048, 8192], BF16)
    Wg = cx.din("Wg", [2048, 1296], F32)
    cwd = cx.din("cw", [768, 5], F32)
    cbd = cx.din("cb", [768, 1], F32)
    hp = cx.din("hp", [3, 16], F32)
    nwd = cx.din("nw", [1, 512], F32)
    ynT = cx.dout("ynT", [512, 8192], BF16)
    hTv = hT.rearrange("(c p) t -> p c t", p=128)
    ynTv = ynT.rearrange("(k p) t -> p k t", p=128)
    cx.consts()
    m_le = cx.mask("m_le", ALU.is_ge, 0, pmul=-1, fmul=1)
    m_gt = cx.mask("m_gt", ALU.is_gt, 0, pmul=1, fmul=-1)
    m_ge = cx.mask("m_ge", ALU.is_ge, 0, pmul=1, fmul=-1)
    m_lt = cx.mask("m_lt", ALU.is_gt, 0, pmul=-1, fmul=1)
    ones = cx.ones32
    sb = cx.sb
    WZ = sb("WZ", [128, 16, 512], BF16)
    Wgv = Wg.rearrange("(c p) n -> p c n", p=128)
    S.dma(WZ[:], Wgv[:, :, 0:512], (), ['WZ'], 'WZ', eng='pool')
    cw = sb("cw", [128, 6, 5], F32)
    cb = sb("cb", [128, 6, 1], F32)
    S.dma(cw[:], cwd.rearrange("(b p) j -> p b j", p=128), (), ['cw'], 'cw')
    S.dma(cb[:], cbd.rearrange("(b p) j -> p b j", p=128), (), ['cb'], 'cb', allow_slow_non_contiguous=True)
    dtb = sb("dtb", [128, 16], F32)
    a_b = sb("a_b", [128, 16], F32)
    Db = sb("Db", [128, 16], F32)
    nwb = sb("nwb", [128, 512], F32)
    S.dma(dtb[:], hp[0, :].partition_broadcast(128), (), ['dtb'], 'dtb')
    S.dma(a_b[:], hp[1, :].partition_broadcast(128), (), ['a_b'], 'a_b')
    S.dma(Db[:], hp[2, :].partition_broadcast(128), (), ['Db'], 'Db')
    S.dma(nwb[:], nwd[0, :].partition_broadcast(128), (), ['nwb'], 'nwb')
    S.act(a_b[:], a_b[:], AF.Exp, ['a_b'], ['a_b'])
    S.ts('dve', a_b[:], a_b[:], -1.0, None, ALU.mult, None, ['a_b'], ['a_b'])

    xs_tok = sb("xs_tok", [128, 32, 512], BF16)
    B_tok = sb("B_tok", [128, 32, 128], BF16)
    BT = sb("BT", [128, 4096], BF16)
    CT = sb("CT", [128, 4096], BF16)
    dt = sb("dt", [128, 32, 16], F32)
    adt = sb("adt", [128, 32, 16], F32)
    ecs = sb("ecs", [128, 32, 16], F32)
    edec = sb("edec", [128, 32, 16], F32)
    etot = sb("etot", [128, 32, 16], F32)
    Sin_f = sb("Sin_f", [128, 32, 512], BF16)
    Sf = sb("Sf", [128, 512], F32)
    Sb = sb("Sb", [128, 512], F32)
    Sb16 = sb("Sb16", [128, 512], BF16)
    dd = sb("dd", [128, 8], F32)
    xdtd = sb("xdtd", [128, 512], BF16)
    P = cx.ps

    def b64(ap):
        return ap.unsqueeze(2).broadcast_to([128, 8, 64])

    def h64(ap):
        return ap.rearrange("p (h d) -> p h d", d=64)

    for b in range(2):
        tb = b * 4096
        with contextlib.ExitStack() as es:
            WA = sb("WA", [128, 16, 784], BF16, es)
            S.dma(WA[:], Wgv[:, :, 512:1296], (), ['WA'], 'WA', eng='pool')
            hwin = [sb(f"hwin{i}", [128, 16, 516], BF16, es) for i in range(2)]
            xb4 = sb("xb4", [128, 4, 16], F32, es)
            dd4 = sb("dd4", [128, 4, 8], F32, es)
            xdtd2 = [sb(f"xdtd2_{i}", [128, 512], BF16, es) for i in range(2)]
            u = sb("u", [128, 6, 516], F32, es)
            acc = [sb(f"acc{i}", [128, 512], F32, es) for i in range(2)]
            xsT = [sb(f"xsT{i}", [128, 512], BF16, es) for i in range(2)]
            xb_ = sb("xb_", [128, 16], F32, es)
            S.memset('dve', Sf[:], 0.0, ['Sf'])
            for sc in range(8):
                hw, hwn = hwin[sc % 2], f"hwin{sc % 2}"
                lo, hi = (2 if sc == 0 else 0), (514 if sc == 7 else 516)
                if sc == 0:
                    S.memset('pool', hw[:, :, 0:2], 0.0, [hwn])
                if sc == 7:
                    S.memset('pool', hw[:, :, 514:516], 0.0, [hwn])
                S.dma(hw[:, :, lo:hi], hTv[:, :, tb + sc * 512 - 2 + lo: tb + sc * 512 - 2 + hi], (), [hwn], hwn)
                for blk in range(6):
                    ps, pn = P[blk % 2], f"ps{blk % 2}"
                    for k in range(16):
                        S.mm(ps[:], WA[:, k, blk * 128:(blk + 1) * 128], hw[:, k, 2:514], k == 0, k == 15, ['WA', hwn], [pn])
                    S.copy('act', u[:, blk, 2:514], ps[:], [pn], ['u'])
                    for side in range(2):
                        cols = slice(0, 2) if side == 0 else slice(514, 516)
                        for k in range(16):
                            S.mm(P[2][:, blk * 4 + side * 2: blk * 4 + side * 2 + 2], WA[:, k, blk * 128:(blk + 1) * 128],
                                 hw[:, k, cols], k == 0, k == 15, ['WA', hwn], ['ps2'])
                pv = P[2][:, 0:24].rearrange("p (b f) -> p b f", f=4)
                S.copy('dve', u[:, :, 0:2], pv[:, :, 0:2], ['ps2'], ['u'])
                S.copy('dve', u[:, :, 514:516], pv[:, :, 2:4], ['ps2'], ['u'])
                for blk in range(6):
                    ce = 'dve'
                    a, an = acc[blk % 2], f"acc{blk % 2}"
                    S.ts(ce, a[:], u[:, blk, 0:512], cw[:, blk, 0:1], cb[:, blk, 0:1], ALU.mult, ALU.add, ['u', 'cw', 'cb'], [an])
                    for j in range(1, 5):
                        S.stt(ce, a[:], u[:, blk, j:j + 512], cw[:, blk, j:j + 1], a[:], ALU.mult, ALU.add, ['u', 'cw', an], [an])
                    if blk == 5:
                        S.act(CT[:, sc * 512:(sc + 1) * 512], a[:], AF.Silu, [an], ['CT'])
                        continue
                    if blk == 4:
                        src = BT[:, sc * 512:(sc + 1) * 512]
                        S.act(src, a[:], AF.Silu, [an], ['BT'])
                        srcres = ['BT']
                    else:
                        src = xsT[blk % 2][:]
                        S.act(src, a[:], AF.Silu, [an], [f'xsT{blk % 2}'])
                        srcres = [f'xsT{blk % 2}']
                    pt, ptn = P[3 + blk % 2], f"ps{3 + blk % 2}"
                    ptb = pt[:].bitcast(BF16)
                    for cc in range(4):
                        S.transpose(ptb[:, cc * 128:(cc + 1) * 128], src[:, cc * 128:(cc + 1) * 128], cx.ident16[:],
                                    srcres + ['ident16'], [ptn])
                    if blk == 4:
                        S.copy('dve', B_tok[:, sc * 4:(sc + 1) * 4, :], ptb[:, 0:512].rearrange("p (c n) -> p c n", n=128),
                               [ptn], ['B_tok'])
                    else:
                        S.copy('dve', xs_tok[:, sc * 4:(sc + 1) * 4, blk * 128:(blk + 1) * 128],
                               ptb[:, 0:512].rearrange("p (c n) -> p c n", n=128), [ptn], ['xs_tok'])
                c0 = sc * 4
                for cc in range(4):
                    for k in range(16):
                        S.mm(P[5][:, cc * 16:(cc + 1) * 16], hw[:, k, 2 + cc * 128: 2 + (cc + 1) * 128], WA[:, k, 768:784],
                             k == 0, k == 15, ['WA', hwn], ['ps5'])
                S.tt('dve', xb4[:], P[5][:, 0:64].rearrange("p (c n) -> p c n", n=16),
                     dtb[:].unsqueeze(1).broadcast_to([128, 4, 16]), ALU.add, ['ps5', 'dtb'], ['xb4'])
                S.act(xb4[:], xb4[:], AF.Exp, ['xb4'], ['xb4'])
                S.act(dt[:, c0:c0 + 4, :], xb4[:], AF.Ln, ['xb4', 'ones32'], ['dt'], bias=ones[:, 0:1])
                S.tt('dve', adt[:, c0:c0 + 4, :], dt[:, c0:c0 + 4, :], a_b[:].unsqueeze(1).broadcast_to([128, 4, 16]), ALU.mult,
                     ['dt', 'a_b'], ['adt'])
                for mi, (m, mn) in enumerate([(m_le, 'm_le'), (m_gt, 'm_gt'), (m_ge, 'm_ge'), (m_lt, 'm_lt'), (ones, 'ones32')]):
                    S.mm(P[6][:, mi * 64:(mi + 1) * 64], m[:], adt[:, c0:c0 + 4, :].rearrange("p c n -> p (c n)"), True, True,
                         [mn, 'adt'], ['ps6'])
                P6 = P[6][:, 0:320].rearrange("p (m c n) -> p m c n", m=5, c=4)
                S.act(ecs[:, c0:c0 + 4, 0:8], P6[:, 0, :, 0:8], AF.Exp, ['ps6'], ['ecs'])
                S.act(ecs[:, c0:c0 + 4, 8:16], P6[:, 2, :, 8:16], AF.Exp, ['ps6'], ['ecs'])
                S.act(edec[:, c0:c0 + 4, 0:8], P6[:, 1, :, 0:8], AF.Exp, ['ps6'], ['edec'])
                S.act(edec[:, c0:c0 + 4, 8:16], P6[:, 3, :, 8:16], AF.Exp, ['ps6'], ['edec'])
                S.act(etot[:, c0:c0 + 4, :], P6[:, 4, :, :], AF.Exp, ['ps6'], ['etot'])
                S.tt('dve', dd4[:], dt[:, c0:c0 + 4, 0:8], edec[:, c0:c0 + 4, 0:8], ALU.mult, ['dt', 'edec'], ['dd4'])
                for cc in range(4):
                    c = c0 + cc
                    xd, xdn = xdtd2[cc % 2], f"xdtd2_{cc % 2}"
                    S.tt('pool', h64(xd[:]), h64(xs_tok[:, c, :]), b64(dd4[:, cc, :]), ALU.mult, ['xs_tok', 'dd4'], [xdn])
                    S.mm(P[7][:], B_tok[:, c, :], xd[:], True, True, ['B_tok', xdn], ['ps7'])
                    S.copy('act', Sin_f[:, c, :], Sf[:], ['Sf'], ['Sin_f'])
                    S.tt('dve', h64(Sf[:]), h64(Sf[:]), b64(etot[:, c, 0:8]), ALU.mult, ['Sf', 'etot'], ['Sf'])
                    S.tt('dve', Sf[:], Sf[:], P[7][:], ALU.add, ['Sf', 'ps7'], ['Sf'])
        S.barrier()
        with contextlib.ExitStack() as es:
            def two(name, shape, dtp):
                return [sb(f"{name}{i}", shape, dtp, es) for i in range(2)]
            hz = two("hz", [128, 16, 128], BF16)
            sz = two("sz", [128, 512], F32)
            CBm = [two(f"CBm{d}_", [128, 128], F32) for d in range(2)]
            A = [two(f"A{d}_", [128, 8, 128], F32) for d in range(2)]
            E = [[sb(f"E{d}{q}", [128, 4, 128], F32, es) for q in range(2)] for d in range(2)]
            MT = [two(f"MT{d}_", [128, 8, 128], BF16) for d in range(2)]
            xdt = [two(f"xdt{d}_", [128, 512], BF16) for d in range(2)]
            y = two("y", [128, 512], F32)
            t2 = two("t2", [128, 512], F32)
            t3 = two("t3", [128, 512], F32)
            yn = two("yn", [128, 512], BF16)
            junk = two("junk", [128, 512], F32)
            ssq = two("ssq", [128, 1], F32)
            rstd = two("rstd", [128, 1], F32)
            xdb = two("xdb", [128, 512], BF16)
            ddb = two("ddb", [128, 8], F32)
            yo = two("yo", [128, 4, 512], BF16)
            S.memset('dve', Sb[:], 0.0, ['Sb'])
            S.memset('dve', Sb16[:], 0.0, ['Sb16'])

            def stage_a(c):
                p = c % 2
                hzz, hzn = hz[p], f"hz{p}"
                S.dma(hzz[:], hTv[:, :, tb + c * 128: tb + (c + 1) * 128], (), [hzn], hzn)
                csl = slice(c * 128, (c + 1) * 128)
                S.mm(P[1][:, 0:128], BT[:, csl], CT[:, csl], True, True, ['BT', 'CT'], ['ps1'])
                for d in range(2):
                    mask2, m2n = (m_le, 'm_le') if d == 0 else (m_ge, 'm_ge')
                    S.tt('pool', A[d][p][:], mask2[:].unsqueeze(1).broadcast_to([128, 8, 128]),
                         adt[:, c, d * 8:(d + 1) * 8].unsqueeze(2).broadcast_to([128, 8, 128]), ALU.mult, [m2n, 'adt'], [f'A{d}{p}'])
                S.tt('dve', CBm[0][p][:], P[1][:, 0:128], m_le[:], ALU.mult, ['ps1', 'm_le'], [f'CBm0{p}'])
                S.tt('dve', CBm[1][p][:], P[1][:, 0:128], m_ge[:], ALU.mult, ['ps1', 'm_ge'], [f'CBm1{p}'])
                for d in range(2):
                    S.tt('dve', h64(xdt[d][p][:]), h64(xs_tok[:, c, :]), b64(dt[:, c, d * 8:(d + 1) * 8]), ALU.mult,
                         ['xs_tok', 'dt'], [f'xdt{d}{p}'])
                S.tt('dve', ddb[p][:], dt[:, c, 8:16], edec[:, c, 8:16], ALU.mult, ['dt', 'edec'], [f'ddb{p}'])
                S.tt('pool', h64(xdb[p][:]), h64(xs_tok[:, c, :]), b64(ddb[p][:]), ALU.mult, ['xs_tok', f'ddb{p}'], [f'xdb{p}'])
                S.tt('pool', h64(t3[p][:]), h64(xs_tok[:, c, :]), b64(Db[:, 0:8]), ALU.mult, ['xs_tok', 'Db'], [f't3{p}'])
                for q in range(2):
                    for d in range(2):
                        Lm, lmn = (m_gt, 'm_gt') if d == 0 else (m_lt, 'm_lt')
                        pd, pdn = P[2 + d], f"ps{2 + d}"
                        S.mm(pd[:], Lm[:], A[d][p][:, 4 * q:4 * q + 4, :].rearrange("p h l -> p (h l)"), True, True,
                             [lmn, f'A{d}{p}'], [pdn])
                        S.act(E[d][q][:].rearrange("p h l -> p (h l)"), pd[:], AF.Exp, [pdn], [f'E{d}{q}'])
                    for d in range(2):
                        S.tt('pool', MT[d][p][:, 4 * q:4 * q + 4, :], E[d][q][:],
                             CBm[d][p][:].unsqueeze(1).broadcast_to([128, 4, 128]), ALU.mult, [f'E{d}{q}', f'CBm{d}{p}'],
                             [f'MT{d}{p}'])
                for k in range(16):
                    S.mm(P[0][:], hzz[:, k, :], WZ[:, k, :], k == 0, k == 15, [hzn, 'WZ'], ['ps0'])
                S.act(sz[p][:], P[0][:], AF.Silu, ['ps0'], [f'sz{p}'])

            def stage_b(c):
                p = c % 2
                csl = slice(c * 128, (c + 1) * 128)
                for h in range(8):
                    hs = slice(h * 64, (h + 1) * 64)
                    S.mm(P[4][:, hs], MT[0][p][:, h, :], xdt[0][p][:, hs], True, False, [f'MT0{p}', f'xdt0{p}'], ['ps4'])
                    S.mm(P[4][:, hs], MT[1][p][:, h, :], xdt[1][p][:, hs], False, True, [f'MT1{p}', f'xdt1{p}'], ['ps4'])
                S.mm(P[5][:], CT[:, csl], Sin_f[:, c, :], True, True, ['CT', 'Sin_f'], ['ps5'])
                S.mm(P[6][:], CT[:, csl], Sb16[:], True, True, ['CT', 'Sb16'], ['ps6'])
                S.mm(P[7][:], B_tok[:, c, :], xdb[p][:], True, True, ['B_tok', f'xdb{p}'], ['ps7'])
                S.tt('dve', h64(Sb[:]), h64(Sb[:]), b64(etot[:, c, 8:16]), ALU.mult, ['Sb', 'etot'], ['Sb'])
                S.tt('dve', Sb[:], Sb[:], P[7][:], ALU.add, ['Sb', 'ps7'], ['Sb'])
                S.copy('act', Sb16[:], Sb[:], ['Sb'], ['Sb16'])
                yy, yyn = y[p], f'y{p}'
                S.tt('dve', h64(yy[:]), h64(P[5][:]), b64(ecs[:, c, 0:8]), ALU.mult, ['ps5', 'ecs'], [yyn])
                S.tt('dve', h64(t2[p][:]), h64(P[6][:]), b64(ecs[:, c, 8:16]), ALU.mult, ['ps6', 'ecs'], [f't2{p}'])
                S.tt('pool', t2[p][:], t2[p][:], t3[p][:], ALU.add, [f't2{p}', f't3{p}'], [f't2{p}'])
                S.tt('dve', yy[:], yy[:], P[4][:], ALU.add, [yyn, 'ps4'], [yyn])
                S.tt('pool', yy[:], yy[:], t2[p][:], ALU.add, [yyn, f't2{p}'], [yyn])
                S.tt('pool', yy[:], yy[:], sz[p][:], ALU.mult, [yyn, f'sz{p}'], [yyn])
                S.act(junk[p][:], yy[:], AF.Square, [yyn], [f'junk{p}', f'ssq{p}'], accum_out=ssq[p][:])
                S.ts('dve', rstd[p][:], ssq[p][:], 1.0 / 512, EPS, ALU.mult, ALU.add, [f'ssq{p}'], [f'rstd{p}'])
                S.act(rstd[p][:], rstd[p][:], AF.Sqrt, [f'rstd{p}'], [f'rstd{p}'])
                S.op('dve', lambda e, p=p: e.reciprocal(rstd[p][:], rstd[p][:]), [f'rstd{p}'], [f'rstd{p}'])
                S.stt('dve', yn[p][:], yy[:], rstd[p][:], nwb[:], ALU.mult, ALU.mult, [yyn, f'rstd{p}', 'nwb'], [f'yn{p}'])
                ptb = P[7][:].bitcast(BF16)
                for blk in range(4):
                    S.transpose(ptb[:, blk * 128:(blk + 1) * 128], yn[p][:, blk * 128:(blk + 1) * 128], cx.ident16[:],
                                [f'yn{p}', 'ident16'], ['ps7'])
                sc = c // 4
                yoo, yon = yo[sc % 2], f"yo{sc % 2}"
                S.copy('act', yoo[:, :, (c % 4) * 128:(c % 4 + 1) * 128], ptb[:, 0:512].rearrange("p (k t) -> p k t", t=128),
                       ['ps7'], [yon])
                if c % 4 == 0:
                    S.dma(ynTv[:, :, tb + sc * 512: tb + (sc + 1) * 512], yoo[:], [yon], ['ynT'], f'o{yon}')

            stage_a(31)
            for c in range(31, -1, -1):
                if c > 0:
                    stage_a(c - 1)
                stage_b(c)
        S.barrier()
    return cx.finish(['ynT'])


def build_na():
    cx = Ctx()
    S = cx.S
    sb = cx.sb
    P = cx.ps
    hT = cx.din("hT", [2048, 8192], BF16)
    Wqd = cx.din("Wq", [2048, 768], F32)
    biasd = cx.din("bias", [2, 15, 128, 64], F32)
    oT = cx.dout("oT", [256, 8192], BF16)
    hTv = hT.rearrange("(c p) t -> p c t", p=128)
    oTv = oT.rearrange("(k p) t -> p k t", p=128)
    cx.consts()
    QT = sb("QT", [128, 2, 8192], BF16)
    KT = sb("KT", [128, 2, 8192], BF16)
    V64 = sb("V64", [64, 128, 256], BF16)
    NBIG = 30000.0
    with contextlib.ExitStack() as es:
        Wq = sb("Wq", [128, 16, 768], BF16, es)
        S.dma(Wq[:], Wqd.rearrange("(c p) n -> p c n", p=128), (), ['Wq'], 'Wq', eng='pool')
        hwl = [sb(f"hw{i}", [128, 16, 512], BF16, es) for i in range(2)]
        it = 0
        for blk in range(16):
            hw, hwn = hwl[blk % 2], f"hw{blk % 2}"
            S.dma(hw[:], hTv[:, :, blk * 512:(blk + 1) * 512], (), [hwn], hwn)
            for pair in range(2):
                for which in range(2):
                    ps, pn = P[it % 4], f"ps{it % 4}"
                    it += 1
                    col = which * 256 + pair * 128
                    for k in range(16):
                        S.mm(ps[:], Wq[:, k, col:col + 128], hw[:, k, :], k == 0, k == 15, ['Wq', hwn], [pn])
                    if which == 0:
                        S.act(QT[:, pair, blk * 512:(blk + 1) * 512], ps[:], AF.Identity, [pn], ['QT'], scale=0.125)
                    else:
                        S.copy('dve', KT[:, pair, blk * 512:(blk + 1) * 512], ps[:], [pn], ['KT'])
            for tt in range(8):
                ps, pn = P[4 + (tt % 4)], f"ps{4 + (tt % 4)}"
                for k in range(16):
                    S.mm(ps[0:64, 0:256], hw[:, k, tt * 64:(tt + 1) * 64], Wq[:, k, 512:768], k == 0, k == 15, ['Wq', hwn], [pn])
                S.copy('act' if tt % 2 else 'dve', V64[:, blk * 8 + tt, :], ps[0:64, 0:256], [pn], ['V64'])
    S.barrier()
    wa = [sb(f"wa{i}", [128, 64], F32) for i in range(4)]
    for half in range(2):
        psl = slice(half * 64, (half + 1) * 64)
        specs = [([[1, 64]], 8, -1, ALU.is_ge),
                 ([[1, 64]], -48, 0, ALU.is_ge),
                 ([[-1, 64]], 7, 1, ALU.is_ge),
                 ([[-1, 64]], 15, 0, ALU.is_ge)]
        for i, (pat, base, cm, op) in enumerate(specs):
            S.op('pool', lambda e, i=i, pat=pat, base=base, cm=cm, op=op, psl=psl: e.affine_select(
                wa[i][psl, :], cx.ones32[psl, 0:64], pat, op, 0.0, base=base, channel_multiplier=cm), ['ones32'], [f'wa{i}'])
    S.tt('dve', wa[0][:], wa[0][:], wa[1][:], ALU.max, ['wa0', 'wa1'], ['wa0'])
    S.tt('dve', wa[2][:], wa[2][:], wa[3][:], ALU.max, ['wa2', 'wa3'], ['wa2'])
    S.tt('dve', wa[0][:], wa[0][:], wa[2][:], ALU.mult, ['wa0', 'wa2'], ['wa0'])
    S.ts('dve', wa[0][:], wa[0][:], NBIG, -NBIG, ALU.mult, ALU.add, ['wa0'], ['wa0'])
    biasv = sb("biasv", [128, 2, 8, 512], F32)
    with contextlib.ExitStack() as es:
        braw = sb("braw", [128, 2, 15, 64], F32, es)
        for pair in range(2):
            S.dma(braw[:, pair], biasd[pair].rearrange("d p k -> p d k"), (), ['braw'], f'braw{pair}')
        for pair in range(2):
            for v in range(8):
                S.tt('dve', biasv[:, pair, v, :].rearrange("p (a k) -> p a k", k=64), braw[:, pair, v:v + 8, :],
                     wa[0][:].unsqueeze(1).broadcast_to([128, 8, 64]), ALU.add, ['braw', 'wa0'], ['biasv'])
        S.barrier()
    NCH = 4
    sc_ = [sb(f"sc{i}", [128, 512], F32) for i in range(NCH)]
    p32 = [sb(f"p32{i}", [128, 512], F32) for i in range(NCH)]
    pnb = [sb(f"pnb{i}", [128, 512], BF16) for i in range(NCH)]
    PT = [sb(f"PT{i}", [64, 8, 128], BF16) for i in range(NCH)]
    mx = [sb(f"mx{i}", [128, 1], F32) for i in range(NCH)]
    ssum = [sb(f"ssum{i}", [128, 1], F32) for i in range(NCH)]
    ob = [sb(f"ob{i}", [128, 2, 512], BF16) for i in range(2)]
    for b in range(2):
        tb = b * 4096
        for rr in range(0, 64, 2):
            chains = []
            for r in (rr, rr + 1):
                for pair in range(2):
                    chains.append((r, pair))
            info = []
            for ci, (r, pair) in enumerate(chains):
                r0 = min(max(r - 4, 0), 56)
                info.append(dict(r=r, pair=pair, v=r0 - r + 7, tq=tb + r * 64, tk=tb + r0 * 64, i=ci))
            obb, obn = ob[(rr // 8) % 2], f"ob{(rr // 8) % 2}"
            for c_ in info:
                i, pair = c_['i'], c_['pair']
                psS, psn = P[i], f"ps{i}"
                for hh in range(2):
                    hs = slice(hh * 64, (hh + 1) * 64)
                    S.mm(psS[hs, :], QT[hs, pair, c_['tq']:c_['tq'] + 64], KT[hs, pair, c_['tk']:c_['tk'] + 512], True, True,
                         ['QT', 'KT'], [psn])
            for c_ in info:
                i = c_['i']
                S.tt('dve', sc_[i][:], P[i][:], biasv[:, c_['pair'], c_['v'], :], ALU.add, [f"ps{i}", 'biasv'], [f"sc{i}"])
            for c_ in info:
                i = c_['i']
                S.op('dve', lambda e, i=i: e.tensor_reduce(mx[i][:], sc_[i][:], AX.X, ALU.max), [f"sc{i}"], [f"mx{i}"])
            for c_ in info:
                i = c_['i']
                S.ts('dve', mx[i][:], mx[i][:], -1.0, None, ALU.mult, None, [f"mx{i}"], [f"mx{i}"])
            for c_ in info:
                i = c_['i']
                S.act(p32[i][:], sc_[i][:], AF.Exp, [f"sc{i}", f"mx{i}"], [f"p32{i}", f"ssum{i}"], bias=mx[i][:], accum_out=ssum[i][:])
            for c_ in info:
                i = c_['i']
                S.op('dve', lambda e, i=i: e.reciprocal(ssum[i][:], ssum[i][:]), [f"ssum{i}"], [f"ssum{i}"])
            for c_ in info:
                i = c_['i']
                S.ts('dve', pnb[i][:], p32[i][:], ssum[i][:], None, ALU.mult, None, [f"p32{i}", f"ssum{i}"], [f"pnb{i}"])
            for c_ in info:
                i = c_['i']
                psT, ptn = P[4 + i % 2], f"ps{4 + i % 2}"
                ptb = psT[:].bitcast(BF16)
                for c in range(8):
                    S.transpose(ptb[0:64, c * 128:(c + 1) * 128], pnb[i][:, c * 64:(c + 1) * 64], cx.ident16[:],
                                [f"pnb{i}", 'ident16'], [ptn])
                S.copy('act', PT[i][:].rearrange("p c q -> p (c q)"), ptb[0:64, :], [ptn], [f"PT{i}"])
            for c_ in info:
                i, pair, r = c_['i'], c_['pair'], c_['r']
                psO, pon = P[6 + i % 2], f"ps{6 + i % 2}"
                for hh in range(2):
                    hs = slice(hh * 64, (hh + 1) * 64)
                    hc = (pair * 2 + hh) * 64
                    for c in range(8):
                        S.mm(psO[hs, 0:64], V64[:, c_['tk'] // 64 + c, hc:hc + 64], PT[i][:, c, hs], c == 0, c == 7,
                             ['V64', f"PT{i}"], [pon])
                S.copy('act', obb[:, pair, (r % 8) * 64:(r % 8 + 1) * 64], psO[:, 0:64], [pon], [obn])
            if (rr + 1) % 8 == 7:
                S.dma(oTv[:, :, tb + (rr // 8) * 512: tb + (rr // 8 + 1) * 512], obb[:], [obn], ['oT'], f'o{obn}')
    return cx.finish(['oT'])


_PROGS = {}


def _prog(key, fn):
    if key not in _PROGS:
        _PROGS[key] = fn()
    return _PROGS[key]


def _run(nc, in_maps):
    res = run_bass_kernel_spmd(nc, in_maps, core_ids=list(range(8)))
    return res.results


def kernel(x, c, ada_w, ada_b, norm_mix, norm_ffn, ssd_w_in, ssd_conv_w, ssd_conv_b, ssd_a_log,
           ssd_dt_bias, ssd_d, ssd_norm_w, ssd_w_out, na_w_qkv, na_rpb, na_w_o,
           moe_w_group, moe_w_expert, moe_w1, moe_w3, moe_w2, final_norm):
    f32 = np.float32
    A = lambda a: np.ascontiguousarray(np.asarray(a, dtype=f32))
    x = A(x); c = A(c); ada_w = A(ada_w); ada_b = A(ada_b)
    xf = x.reshape(8192, 2048)
    cT = np.ascontiguousarray(c.T.reshape(16, 128, 2).transpose(1, 0, 2))
    nc = _prog('ada', build_ada)
    r = _run(nc, [dict(cT=cT, aw=np.ascontiguousarray(ada_w[:, :, j * 1536:(j + 1) * 1536]),
                       ab=np.ascontiguousarray(ada_b[:, j * 1536:(j + 1) * 1536])) for j in range(8)])
    mod = np.concatenate([np.asarray(q['mod']) for q in r], axis=2)
    nm = A(norm_mix); nf = A(norm_ffn); fn_ = A(final_norm)
    nc = _prog('t0', build_t0)
    maps = []
    for i in range(8):
        b = i // 4
        rows = np.stack([mod[0, b, 0:2048], mod[0, b, 2048:4096], nm[0]])
        maps.append(dict(x=np.ascontiguousarray(xf[i * 1024:(i + 1) * 1024]), rows=np.ascontiguousarray(rows)))
    r = _run(nc, maps)
    hT = np.ascontiguousarray(np.concatenate([np.asarray(q['hT']) for q in r], axis=1))
    w_in = A(ssd_w_in)[0]; cw = A(ssd_conv_w)[0]; cbv = A(ssd_conv_b)[0]
    alog = A(ssd_a_log)[0]; dtb = A(ssd_dt_bias)[0]; dsk = A(ssd_d)[0]; snw = A(ssd_norm_w)[0]
    nc = _prog('ssd', build_ssd)
    maps = []
    for g in range(8):
        cols = np.concatenate([np.arange(g * 512, (g + 1) * 512), 4096 + np.arange(g * 512, (g + 1) * 512),
                               8192 + np.arange(g * 128, (g + 1) * 128), 9216 + np.arange(g * 128, (g + 1) * 128),
                               10240 + np.arange(g * 8, (g + 1) * 8), 10304 + np.arange(g * 8, (g + 1) * 8)])
        ch = np.concatenate([np.arange(g * 512, (g + 1) * 512), 4096 + np.arange(g * 128, (g + 1) * 128),
                             5120 + np.arange(g * 128, (g + 1) * 128)])
        hs = slice(g * 8, (g + 1) * 8)
        hp = np.zeros((3, 16), f32)
        hp[0, 0:8] = dtb[0, hs]; hp[0, 8:16] = dtb[1, hs]
        hp[1, 0:8] = alog[0, hs]; hp[1, 8:16] = alog[1, hs]
        hp[2, 0:8] = dsk[hs]
        maps.append(dict(hT=hT, Wg=np.ascontiguousarray(w_in[:, cols]), cw=np.ascontiguousarray(cw[:, ch].T),
                         cb=np.ascontiguousarray(cbv[ch][:, None]), hp=hp,
                         nw=np.ascontiguousarray(snw[g * 512:(g + 1) * 512][None, :])))
    r = _run(nc, maps)
    ynT = np.concatenate([np.asarray(q['ynT']) for q in r], axis=0)
    def c_maps(l, xin, mixT, wout, last):
        wr = np.ascontiguousarray(np.concatenate([A(moe_w_group)[l], A(moe_w_expert)[l]], axis=1))
        w1 = A(moe_w1)[l]; w3 = A(moe_w3)[l]; w2 = A(moe_w2)[l]
        maps = []
        for i in range(8):
            b = i // 4
            m = mod[l, b]
            if last:
                nxt = [fn_, fn_, fn_]
            else:
                nxt = [mod[l + 1, b, 0:2048], mod[l + 1, b, 2048:4096], nm[l + 1]]
            rows = np.stack([m[4096:6144], m[6144:8192], m[8192:10240], m[10240:12288], nf[l]] + nxt)
            maps.append(dict(x=np.ascontiguousarray(xin[i * 1024:(i + 1) * 1024]),
                             mixT=np.ascontiguousarray(mixT[:, i * 1024:(i + 1) * 1024]),
                             wout=wout, rows=np.ascontiguousarray(rows), wr=wr, w1=w1, w3=w3, w2=w2))
        return maps
    nc = _prog('c0', lambda: build_c(4096, False))
    r = _run(nc, c_maps(0, xf, ynT, A(ssd_w_out)[0], False))
    x1 = np.concatenate([np.asarray(q['xo']) for q in r], axis=0)
    hT1 = np.ascontiguousarray(np.concatenate([np.asarray(q['hTn']) for q in r], axis=1))
    wqkv = A(na_w_qkv)[0]; rpb = A(na_rpb)[0]
    jq = np.arange(64)[:, None]; kc = np.arange(64)[None, :]
    dx = kc - jq + 15
    ok = (dx >= 0) & (dx <= 30)
    dxc = np.clip(dx, 0, 30)
    nc = _prog('na', build_na)
    maps = []
    for j in range(8):
        Wq = np.concatenate([wqkv[:, w * 2048 + j * 256: w * 2048 + (j + 1) * 256] for w in range(3)], axis=1)
        bias = np.zeros((2, 15, 128, 64), f32)
        for pair in range(2):
            for hh in range(2):
                h = 4 * j + pair * 2 + hh
                g_ = rpb[h][:, dxc]
                g_ = np.where(ok[None], g_, f32(0))
                bias[pair, :, hh * 64:(hh + 1) * 64, :] = g_
        maps.append(dict(hT=hT1, Wq=np.ascontiguousarray(Wq), bias=bias))
    r = _run(nc, maps)
    oT = np.concatenate([np.asarray(q['oT']) for q in r], axis=0)
    nc = _prog('c1', lambda: build_c(2048, True))
    r = _run(nc, c_maps(1, x1, oT, A(na_w_o)[0], True))
    out = np.concatenate([np.asarray(q['out']) for q in r], axis=0)
    return out.reshape(2, 4096, 2048).astype(f32)
```
